# Optimizing a Trainium2 kernel written in Bass

```python
import math
import jax
import jax.numpy as jnp
from jax import lax
import numpy as np

D_MODEL = 4096
BATCH = 4
SEQ = 2048
DEPTH = 1

MIX_WIDTH = D_MODEL
SSM_WIDTH = MIX_WIDTH // 2
SSM_GROUP = 16
SSM_GROUPS = SSM_WIDTH // SSM_GROUP
SSM_STATE = 64
ATTN_WIDTH = MIX_WIDTH - SSM_WIDTH
HEAD_DIM = 128
N_Q_HEADS = ATTN_WIDTH // HEAD_DIM
N_KV_HEADS = 4
Q_PER_KV = N_Q_HEADS // N_KV_HEADS
KV_WIDTH = N_KV_HEADS * HEAD_DIM
IN_WIDTH = SSM_WIDTH + ATTN_WIDTH + 2 * KV_WIDTH
WINDOW = 128
ATTN_BLOCK = 128
N_EXPERT_GROUPS = 8
EXPERTS_PER_GROUP = 8
N_EXPERTS = N_EXPERT_GROUPS * EXPERTS_PER_GROUP
TOP_K = 2
EXPERT_FF = 512
MOE_BLOCK = 128
LN_EPS = 1e-5
DT_MIN = 1e-3
DT_MAX = 1e-1
NEG_INF = -1e30

kernel_name = 'hybrid_s5_swa_hiermoe_deepnorm_adaln'


def layer_norm(x, g, b):
    xf = x.astype(jnp.float32)
    mu = jnp.mean(xf, axis=-1, keepdims=True)
    var = jnp.mean(jnp.square(xf - mu), axis=-1, keepdims=True)
    y = (xf - mu) * lax.rsqrt(var + LN_EPS)
    return (y * g.astype(jnp.float32) + b.astype(jnp.float32)).astype(x.dtype)


def alibi_slopes(n_heads):
    return 2.0 ** (-8.0 * jnp.arange(1, n_heads + 1, dtype=jnp.float32) / n_heads)


def _diag_linear_scan(a, bu):
    def combine(left, right):
        a_l, b_l = left
        a_r, b_r = right
        return a_l * a_r, a_r * b_l + b_r
    _, states = lax.associative_scan(combine, (a, bu), axis=0)
    return states


def s5_bidirectional(u, lam_re, lam_im, log_dt, b_re, b_im, c_re, c_im, d_skip, w_glu, b_glu):
    f32 = jnp.float32
    bsz, seq, _ = u.shape
    uf = u.astype(f32).reshape(bsz, seq, SSM_GROUPS, SSM_GROUP)
    lam = lax.complex(lam_re.astype(f32), lam_im.astype(f32))
    dt = jnp.exp(log_dt.astype(f32))[..., None]
    lam_bar = jnp.exp(lam * dt)
    b_bar = ((lam_bar - 1.0) / lam)[..., None] * lax.complex(b_re.astype(f32), b_im.astype(f32))
    c_mat = lax.complex(c_re.astype(f32), c_im.astype(f32))
    u_sb = jnp.swapaxes(uf, 0, 1)
    y = d_skip.astype(f32).reshape(SSM_GROUPS, SSM_GROUP) * uf
    for direction in range(2):
        bu = jnp.einsum('sbgh,gph->sbgp', u_sb, b_bar[direction])
        a = jnp.broadcast_to(lam_bar[direction], (seq, 1, SSM_GROUPS, SSM_STATE))
        if direction == 1:
            bu = jnp.flip(bu, axis=0)
        states = _diag_linear_scan(a, bu)
        if direction == 1:
            states = jnp.flip(states, axis=0)
        y = y + jnp.real(jnp.einsum('sbgp,ghp->bsgh', states, c_mat[direction]))
    y = y.reshape(bsz, seq, SSM_WIDTH)
    gate = jax.nn.sigmoid(jax.nn.gelu(y) @ w_glu.astype(f32) + b_glu.astype(f32))
    return (y * gate).astype(u.dtype)


def banded_gqa_attention(q, k, v, sink):
    f32 = jnp.float32
    bsz, seq = q.shape[0], q.shape[1]
    nb = seq // ATTN_BLOCK
    qb = q.reshape(bsz, nb, ATTN_BLOCK, N_KV_HEADS, Q_PER_KV, HEAD_DIM)

    def band(t):
        tp = jnp.pad(t, ((0, 0), (ATTN_BLOCK, ATTN_BLOCK), (0, 0), (0, 0)))
        tb = tp.reshape(bsz, nb + 2, ATTN_BLOCK, N_KV_HEADS, HEAD_DIM)
        return jnp.concatenate([tb[:, :-2], tb[:, 1:-1], tb[:, 2:]], axis=2)

    kb, vb = band(k), band(v)
    scores = jnp.einsum('bnqkgd,bnjkd->bnkgqj', qb, kb, preferred_element_type=f32) * (HEAD_DIM ** -0.5)
    qpos = jnp.arange(nb)[:, None] * ATTN_BLOCK + jnp.arange(ATTN_BLOCK)[None, :]
    kpos = (jnp.arange(nb)[:, None] - 1) * ATTN_BLOCK + jnp.arange(3 * ATTN_BLOCK)[None, :]
    dist = jnp.abs(qpos[:, :, None] - kpos[:, None, :])
    valid = (dist <= WINDOW) & (kpos[:, None, :] >= 0) & (kpos[:, None, :] < seq)
    slopes = alibi_slopes(N_Q_HEADS).reshape(N_KV_HEADS, Q_PER_KV)
    bias = -slopes[None, :, :, None, None] * dist.astype(f32)[:, None, None, :, :]
    scores = jnp.where(valid[None, :, None, None], scores + bias[None], NEG_INF)
    sink_col = jnp.broadcast_to(
        sink.astype(f32).reshape(N_KV_HEADS, Q_PER_KV)[None, None, :, :, None, None],
        scores.shape[:-1] + (1,))
    probs = jax.nn.softmax(jnp.concatenate([scores, sink_col], axis=-1), axis=-1)[..., :-1]
    out = jnp.einsum('bnkgqj,bnjkd->bnqkgd', probs.astype(v.dtype), vb)
    return out.reshape(bsz, seq, ATTN_WIDTH)


def hierarchical_moe(h, w_rg, b_rg, w_re, b_re, w_gate, w_up, w_down):
    f32 = jnp.float32
    bsz, seq, d = h.shape
    n_tok = bsz * seq
    xt = h.reshape(n_tok, d)
    g_probs = jax.nn.softmax((xt @ w_rg).astype(f32) + b_rg.astype(f32), axis=-1)
    g_prob, g_idx = lax.top_k(g_probs, 1)
    e_logits = ((xt @ w_re).astype(f32) + b_re.astype(f32)).reshape(n_tok, N_EXPERT_GROUPS, EXPERTS_PER_GROUP)
    e_in_group = jnp.take_along_axis(e_logits, g_idx[:, :, None], axis=1)[:, 0]
    top_logit, top_local = lax.top_k(e_in_group, TOP_K)
    weights = g_prob * jax.nn.softmax(top_logit, axis=-1)
    expert_idx = g_idx * EXPERTS_PER_GROUP + top_local
    n_assign = n_tok * TOP_K
    flat_e = expert_idx.reshape(n_assign)
    flat_tok = jnp.repeat(jnp.arange(n_tok, dtype=jnp.int32), TOP_K)
    flat_w = weights.reshape(n_assign)
    order = jnp.argsort(flat_e)
    sorted_e = flat_e[order]
    counts = jnp.bincount(flat_e, length=N_EXPERTS)
    padded = (counts + MOE_BLOCK - 1) // MOE_BLOCK * MOE_BLOCK
    pad_end = jnp.cumsum(padded)
    pad_start = pad_end - padded
    start = jnp.cumsum(counts) - counts
    dest = pad_start[sorted_e] + (jnp.arange(n_assign) - start[sorted_e])
    n_rows = (n_assign + N_EXPERTS * (MOE_BLOCK - 1) + MOE_BLOCK - 1) // MOE_BLOCK * MOE_BLOCK
    n_blocks = n_rows // MOE_BLOCK
    row_tok = jnp.full((n_rows,), n_tok, jnp.int32).at[dest].set(flat_tok[order])
    row_w = jnp.zeros((n_rows,), f32).at[dest].set(flat_w[order])
    block_expert = jnp.minimum(
        jnp.searchsorted(pad_end, jnp.arange(n_blocks) * MOE_BLOCK, side='right'), N_EXPERTS - 1)
    xt_pad = jnp.concatenate([xt, jnp.zeros((1, d), xt.dtype)], axis=0)
    rows = xt_pad[row_tok].reshape(n_blocks, MOE_BLOCK, d)

    def expert_block(args):
        xb, e = args
        hb = jax.nn.silu(xb @ w_gate[e]) * (xb @ w_up[e])
        return hb @ w_down[e]

    out_rows = lax.map(expert_block, (rows, block_expert)).reshape(n_rows, d)
    y = jax.ops.segment_sum(out_rows.astype(f32) * row_w[:, None], row_tok, num_segments=n_tok + 1)
    return y[:n_tok].reshape(bsz, seq, d).astype(h.dtype)


def setup_inputs(seed: int = 0) -> dict:
    key = jax.random.key(seed)
    ks = jax.random.split(key, 28)
    f32 = jnp.float32
    beta = (8.0 * DEPTH) ** -0.25
    L, D, G, P, H = DEPTH, D_MODEL, SSM_GROUPS, SSM_STATE, SSM_GROUP

    def nrm(k, shape, s):
        return jax.random.normal(k, shape, f32) * s

    return {
        'x': nrm(ks[0], (BATCH, SEQ, D), 1.0),
        'c': nrm(ks[1], (BATCH, D), 1.0),
        'w_ada': nrm(ks[2], (L, D, 6 * D), 0.1 * D ** -0.5),
        'b_ada': nrm(ks[3], (L, 6 * D), 0.02),
        'w_in': nrm(ks[4], (L, D, IN_WIDTH), D ** -0.5),
        'ssm_lam_re': -0.5 + nrm(ks[5], (L, 2, G, P), 0.01),
        'ssm_lam_im': jnp.pi * jnp.arange(P, dtype=f32) + nrm(ks[6], (L, 2, G, P), 0.01),
        'ssm_log_dt': jax.random.uniform(ks[7], (L, 2, G), f32, math.log(DT_MIN), math.log(DT_MAX)),
        'ssm_b_re': nrm(ks[8], (L, 2, G, P, H), (2.0 * H) ** -0.5),
        'ssm_b_im': nrm(ks[9], (L, 2, G, P, H), (2.0 * H) ** -0.5),
        'ssm_c_re': nrm(ks[10], (L, 2, G, H, P), H ** -0.5),
        'ssm_c_im': nrm(ks[11], (L, 2, G, H, P), H ** -0.5),
        'ssm_d': nrm(ks[12], (L, SSM_WIDTH), 1.0),
        'w_glu': nrm(ks[13], (L, SSM_WIDTH, SSM_WIDTH), SSM_WIDTH ** -0.5),
        'b_glu': nrm(ks[14], (L, SSM_WIDTH), 0.02),
        'attn_sink': nrm(ks[15], (L, N_Q_HEADS), 1.0),
        'w_out': nrm(ks[16], (L, MIX_WIDTH, D), beta * MIX_WIDTH ** -0.5),
        'ln1_g': 1.0 + nrm(ks[17], (L, D), 0.02),
        'ln1_b': nrm(ks[18], (L, D), 0.02),
        'w_router_group': nrm(ks[19], (L, D, N_EXPERT_GROUPS), D ** -0.5),
        'b_router_group': nrm(ks[20], (L, N_EXPERT_GROUPS), 0.01),
        'w_router_expert': nrm(ks[21], (L, D, N_EXPERTS), D ** -0.5),
        'b_router_expert': nrm(ks[22], (L, N_EXPERTS), 0.01),
        'w_gate_e': nrm(ks[23], (L, N_EXPERTS, D, EXPERT_FF), D ** -0.5),
        'w_up_e': nrm(ks[24], (L, N_EXPERTS, D, EXPERT_FF), D ** -0.5),
        'w_down_e': nrm(ks[25], (L, N_EXPERTS, EXPERT_FF, D), beta * EXPERT_FF ** -0.5),
        'ln2_g': 1.0 + nrm(ks[26], (L, D), 0.02),
        'ln2_b': nrm(ks[27], (L, D), 0.02),
    }


def reference(x, c, w_ada, b_ada, w_in, ssm_lam_re, ssm_lam_im, ssm_log_dt, ssm_b_re, ssm_b_im,
              ssm_c_re, ssm_c_im, ssm_d, w_glu, b_glu, attn_sink, w_out, ln1_g, ln1_b,
              w_router_group, b_router_group, w_router_expert, b_router_expert,
              w_gate_e, w_up_e, w_down_e, ln2_g, ln2_b):
    alpha = (2.0 * DEPTH) ** 0.25
    bsz, seq, _ = x.shape
    split_pts = [SSM_WIDTH, SSM_WIDTH + ATTN_WIDTH, SSM_WIDTH + ATTN_WIDTH + KV_WIDTH]
    for l in range(DEPTH):
        mod = jax.nn.silu(c) @ w_ada[l] + b_ada[l]
        sh1, sc1, g1, sh2, sc2, g2 = jnp.split(mod[:, None, :], 6, axis=-1)
        h = x * (1.0 + sc1) + sh1
        proj = h @ w_in[l]
        u, q, k, v = jnp.split(proj, split_pts, axis=-1)
        y_ssm = s5_bidirectional(u, ssm_lam_re[l], ssm_lam_im[l], ssm_log_dt[l], ssm_b_re[l], ssm_b_im[l],
                                 ssm_c_re[l], ssm_c_im[l], ssm_d[l], w_glu[l], b_glu[l])
        y_att = banded_gqa_attention(q.reshape(bsz, seq, N_Q_HEADS, HEAD_DIM),
                                     k.reshape(bsz, seq, N_KV_HEADS, HEAD_DIM),
                                     v.reshape(bsz, seq, N_KV_HEADS, HEAD_DIM), attn_sink[l])
        mix = jnp.concatenate([y_ssm, y_att], axis=-1) @ w_out[l]
        x = layer_norm(alpha * x + (1.0 + g1) * mix, ln1_g[l], ln1_b[l])
        h = x * (1.0 + sc2) + sh2
        ffn = hierarchical_moe(h, w_router_group[l], b_router_group[l], w_router_expert[l], b_router_expert[l],
                               w_gate_e[l], w_up_e[l], w_down_e[l])
        x = layer_norm(alpha * x + (1.0 + g2) * ffn, ln2_g[l], ln2_b[l])
    return x
```

```python
import contextlib
import math
import numpy as np
import concourse.bass as bass
import concourse.mybir as mybir
from concourse.bass_utils import run_bass_kernel_spmd

F32 = mybir.dt.float32
F32R = mybir.dt.float32r
I32 = mybir.dt.int32
ALU = mybir.AluOpType
AF = mybir.ActivationFunctionType
AX = mybir.AxisListType

T = 2048
D = 4096
NJ = 32
CAP = 384
ALPHA = 2.0 ** 0.25
EPS = 1e-5
TWO_PI = 2.0 * math.pi


class Sch:
    ENG = ['pe', 'dve', 'act', 'pool', 'sp']

    def __init__(self, nc, nds=12):
        self.nc = nc
        self.e = dict(pe=nc.tensor, dve=nc.vector, act=nc.scalar, pool=nc.gpsimd, sp=nc.sync)
        self.sem = {k: nc.alloc_semaphore(name='c_' + k) for k in self.ENG}
        self.cnt = {k: 0 for k in self.ENG}
        self.dsem = [nc.alloc_semaphore(name='d_%d' % i) for i in range(nds)]
        self.dcnt = [0] * nds
        self.dnext = 0
        self.seen = {k: {} for k in self.ENG}
        self.last_w = {}
        self.readers = {}

    def _wait(self, eng, tok):
        kind, a, v = tok
        if kind == 'c':
            if a == eng and (eng in ('pe', 'sp') or v > self.cnt[eng]):
                return
            key = ('c', a)
            sem = self.sem[a]
        else:
            key = ('d', a)
            sem = self.dsem[a]
        if self.seen[eng].get(key, 0) >= v:
            return
        self.seen[eng][key] = v
        self.e[eng].wait_ge(sem, v)

    def _deps(self, eng, r, w):
        toks = []
        for k in r:
            t = self.last_w.get(k)
            if t is not None:
                toks.append(t)
        for k in w:
            t = self.last_w.get(k)
            if t is not None:
                toks.append(t)
            toks.extend(self.readers.get(k, {}).values())
        for t in toks:
            self._wait(eng, t)

    def _record(self, tok, r, w):
        for k in r:
            d = self.readers.setdefault(k, {})
            d[(tok[0], tok[1])] = tok
        for k in w:
            self.last_w[k] = tok
            self.readers[k] = {}

    def op(self, eng, fn, r=(), w=(), sig=True):
        self._deps(eng, r, w)
        ins = fn(self.e[eng])
        if sig:
            self.cnt[eng] += 1
            ins.then_inc(self.sem[eng], 1)
            tok = ('c', eng, self.cnt[eng])
        else:
            tok = ('c', eng, self.cnt[eng] + 1)
        self._record(tok, r, w)
        return ins

    def dma(self, eng, out, in_, r=(), w=(), **kw):
        if eng == 'pool':
            eng = 'sp'
        self._deps(eng, r, w)
        i = self.dnext
        self.dnext = (self.dnext + 1) % len(self.dsem)
        if self.dcnt[i] > 0:
            self._wait(eng, ('d', i, self.dcnt[i]))
        ins = self.e[eng].dma_start(out=out, in_=in_, **kw)
        self.dcnt[i] += 16
        ins.then_inc(self.dsem[i], 16)
        tok = ('d', i, self.dcnt[i])
        self._record(tok, r, w)
        return ins

    def barrier(self):
        for eng in self.ENG:
            for a in self.ENG:
                if a != eng and self.cnt[a] > 0:
                    self._wait(eng, ('c', a, self.cnt[a]))
            for i, v in enumerate(self.dcnt):
                if v > 0:
                    self._wait(eng, ('d', i, v))
        self.last_w = {}
        self.readers = {}

    def finish(self):
        for i, v in enumerate(self.dcnt):
            if v > 0:
                self._wait('sp', ('d', i, v))
        for a in self.ENG:
            if a != 'sp' and self.cnt[a] > 0:
                self._wait('sp', ('c', a, self.cnt[a]))


class _Stop(Exception):
    pass


def build(upto=7, debug=False, start=1, sub=0):
    nc = bass.Bass("TRN2", target_bir_lowering=False)
    nc.dge_precook = False
    s = Sch(nc)

    def din(name, shape, dt=F32, used=(1, 2, 3, 4, 5, 6, 7)):
        if not any(start <= p <= upto for p in used):
            return None
        return nc.dram_tensor(name, shape, dt, kind="ExternalInput").ap()

    def dscr(name, shape, dt=F32, made=1, used=(7,)):
        if (max(made) if isinstance(made, tuple) else made) < start:
            if not any(start <= p <= upto for p in used):
                return None
            return nc.dram_tensor(name, shape, dt, kind="ExternalInput").ap()
        return nc.dram_tensor(name, shape, dt, kind="ExternalOutput" if debug else "Internal").ap()

    def ddbg(name, shape, dt=F32):
        return nc.dram_tensor(name, shape, dt, kind="ExternalOutput").ap() if debug else None

    x = din("x", [T, D], used=(2, 5))
    cvec = din("c", [32, 128], used=(1,))
    w_ada = din("w_ada", [D, 6 * D], F32R, used=(1,))
    b_ada = din("b_ada", [192, 128])
    w_in = din("w_in", [D, 5120], F32R, used=(2,))
    lam_re = din("lam_re", [2, 128, 64], used=(3,))
    lam_im = din("lam_im", [2, 128, 64], used=(3,))
    log_dt = din("log_dt", [2, 128], used=(3,))
    b_re = din("b_re", [2, 128, 64, 16], used=(3,))
    b_im = din("b_im", [2, 128, 64, 16], used=(3,))
    c_re = din("c_re", [2, 128, 16, 64], used=(3,))
    c_im = din("c_im", [2, 128, 16, 64], used=(3,))
    ssm_d = din("ssm_d", [16, 128])
    w_glu = din("w_glu", [2048, 2048], F32R, used=(5,))
    b_glu = din("b_glu", [16, 128])
    sink = din("sink", [1, 16], used=(4,))
    w_out = din("w_out", [D, D], F32R, used=(5,))
    ln1_g = din("ln1_g", [32, 128])
    ln1_b = din("ln1_b", [32, 128])
    w_r = din("w_r", [D, 72], used=(5,))
    b_r = din("b_r", [1, 72], used=(5,))
    w_gate = din("w_gate", [64, D, 512], F32R, used=(6,))
    w_up = din("w_up", [64, D, 512], F32R, used=(6,))
    w_down = din("w_down", [64, 512, D], F32R, used=(6,))
    ln2_g = din("ln2_g", [32, 128])
    ln2_b = din("ln2_b", [32, 128])
    out = nc.dram_tensor("out", [T, D], F32, kind="ExternalOutput").ap()

    modrow = dscr("modrow", [192, 128], made=1, used=(1, 2, 3, 4, 5, 6, 7))
    projT = dscr("projT", [5120, T], F32R, made=2, used=(3, 4))
    ymixT = dscr("ymixT", [4096, T], made=(3, 4), used=(5,))
    x1T = dscr("x1T", [D, T], made=5, used=(7,))
    h2tm = dscr("h2tm", [T, D], F32R, made=5, used=(6,))
    yall = dscr("yall", [8 * CAP, D], F32R, made=6, used=(7,))

    dbg_modT = ddbg("dbg_modT", [128, 192])
    dbg_lb = ddbg("dbg_lb", [128, 256])
    dbg_G = ddbg("dbg_G", [128, 128])
    dbg_W = ddbg("dbg_W", [128, 128])
    dbg_pos = ddbg("dbg_pos", [128, 16])

    def phase(n):
        if start <= n <= upto:
            with contextlib.ExitStack() as st_:
                yield st_

    def sub_stop(k):
        if sub == k:
            s.barrier()
            raise _Stop()

    def stop_after(n):
        if upto == n:
            raise _Stop()

    rr = [0]

    def evac_eng():
        rr[0] ^= 1
        return 'act' if rr[0] else 'dve'

    def copy_on(eng, out_ap, in_ap, r, w):
        if eng == 'act':
            s.op('act', lambda e: e.copy(out=out_ap, in_=in_ap), r=r, w=w)
        else:
            s.op(eng, lambda e: e.tensor_copy(out=out_ap, in_=in_ap), r=r, w=w)

    try:
      with contextlib.ExitStack() as gst:
          def gsb(name, shape, dt=F32):
              return gst.enter_context(nc.sbuf_tensor(name, shape, dt))

          ident = gsb("ident", [128, 128])
          onesF = gsb("onesF", [128, 128])
          onesM = gsb("onesM", [128, 128])
          onesR = gsb("onesR", [128, 128], F32R)
          ustrict = gsb("ustrict", [128, 128])
          blockmask = gsb("blockmask", [128, 128])
          rowmask = gsb("rowmask", [128, 8])
          colmask = gsb("colmask", [128, 8, 128])
          sel8 = gsb("sel8", [8, 8, 128])
          iotaS = gsb("iotaS", [128, CAP])
          iotaP = gsb("iotaP", [128, 3])
          modT = gsb("modT", [128, 192])
          badT = gsb("badT", [128, 192])
          sc1p = gsb("sc1p", [128, 32]); g1p = gsb("g1p", [128, 32])
          sc2p = gsb("sc2p", [128, 32]); g2p = gsb("g2p", [128, 32])
          ln1gT = gsb("ln1gT", [128, 32]); ln1bT = gsb("ln1bT", [128, 32])
          ln2gT = gsb("ln2gT", [128, 32]); ln2bT = gsb("ln2bT", [128, 32])
          bgluT = gsb("bgluT", [128, 16]); dT = gsb("dT", [128, 16])
          Gall = gsb("Gall", [128, 16, 8]); WTall = gsb("WTall", [128, 16, 8])
          posall = gsb("posall", [128, 16])

          s.op('pool', lambda e: e.memset(ident[:], 0.0), w=['ident'])
          s.op('pool', lambda e: e.affine_select(out=ident[:], in_=ident[:], pattern=[[-1, 128]],
                                                 compare_op=ALU.not_equal, fill=1.0, base=0, channel_multiplier=1),
               r=['ident'], w=['ident'])
          s.op('pool', lambda e: e.memset(onesF[:], 1.0), w=['onesF'])
          s.op('pool', lambda e: e.memset(onesM[:], 1.0 / D), w=['onesM'])
          s.op('dve', lambda e: e.tensor_copy(out=onesR[:], in_=onesF[:]), r=['onesF'], w=['onesR'])
          s.op('pool', lambda e: e.memset(ustrict[:], 1.0), w=['ustrict'])
          s.op('pool', lambda e: e.affine_select(out=ustrict[:], in_=ustrict[:], pattern=[[1, 128]],
                                                 compare_op=ALU.is_gt, fill=0.0, base=0, channel_multiplier=-1),
               r=['ustrict'], w=['ustrict'])
          s.op('pool', lambda e: e.memset(rowmask[:], 1.0), w=['rowmask'])
          s.op('pool', lambda e: e.affine_select(out=rowmask[:], in_=rowmask[:], pattern=[[-16, 8]],
                                                 compare_op=ALU.is_ge, fill=0.0, base=0, channel_multiplier=1),
               r=['rowmask'], w=['rowmask'])
          s.op('pool', lambda e: e.affine_select(out=rowmask[:], in_=rowmask[:], pattern=[[16, 8]],
                                                 compare_op=ALU.is_ge, fill=0.0, base=15, channel_multiplier=-1),
               r=['rowmask'], w=['rowmask'])
          s.op('pool', lambda e: e.memset(colmask[:], 1.0), w=['colmask'])
          s.op('pool', lambda e: e.affine_select(out=colmask[:], in_=colmask[:], pattern=[[-16, 8], [1, 128]],
                                                 compare_op=ALU.is_ge, fill=0.0, base=0, channel_multiplier=0),
               r=['colmask'], w=['colmask'])
          s.op('pool', lambda e: e.affine_select(out=colmask[:], in_=colmask[:], pattern=[[16, 8], [-1, 128]],
                                                 compare_op=ALU.is_ge, fill=0.0, base=15, channel_multiplier=0),
               r=['colmask'], w=['colmask'])
          with nc.sbuf_tensor("bm_tmp", [128, 8, 128], F32) as bmt:
              s.op('pool', lambda e: e.tensor_tensor(out=bmt[:], in0=colmask[:],
                                                     in1=rowmask[:].unsqueeze(2).to_broadcast([128, 8, 128]), op=ALU.mult),
                   r=['colmask', 'rowmask'], w=['bmt'])
              s.op('dve', lambda e: e.tensor_reduce(out=blockmask[:], in_=bmt[:].rearrange("p g q -> p q g"),
                                                    axis=AX.X, op=ALU.add), r=['bmt'], w=['blockmask'])
              s.barrier()
          s.op('pool', lambda e: e.memset(sel8[:], 1.0), w=['sel8'])
          s.op('pool', lambda e: e.affine_select(out=sel8[:], in_=sel8[:], pattern=[[-1, 8], [0, 128]],
                                                 compare_op=ALU.is_equal, fill=0.0, base=0, channel_multiplier=1),
               r=['sel8'], w=['sel8'])
          s.op('pool', lambda e: e.iota(iotaS[:], pattern=[[1, CAP]], base=0, channel_multiplier=0,
                                        allow_small_or_imprecise_dtypes=True), w=['iotaS'])
          s.op('pool', lambda e: e.iota(iotaP[:], pattern=[[128, 3]], base=0, channel_multiplier=1,
                                        allow_small_or_imprecise_dtypes=True), w=['iotaP'])

          with contextlib.ExitStack() as st:
              def sb(name, shape, dt=F32):
                  return st.enter_context(nc.sbuf_tensor(name, shape, dt))

              def ps(name, shape, dt=F32):
                  return st.enter_context(nc.psum_tensor(name, shape, dt))
              stg = sb("p1_stg", [128, 128])
              pT = ps("p1_pT", [128, 128])
              cT = sb("p1_cT", [128, 32], F32R)
              pm = ps("p1_pm", [1, 256])
              mrow = sb("p1_mrow", [1, 256])
              wa = [sb("p1_wa%d" % i, [128, 32, 256], F32R) for i in range(2)]

              def load_vecT(dst, src, rows, func=None, dstkey=None):
                  s.dma('sp', stg[0:rows, :], src, w=['stg'])
                  s.op('pe', lambda e: e.transpose(pT[:, 0:rows], stg[0:rows, :], ident[0:rows, 0:rows]),
                       r=['stg', 'ident'], w=['pT'])
                  if func is None:
                      s.op('dve', lambda e: e.tensor_copy(out=dst, in_=pT[:, 0:rows]), r=['pT'], w=[dstkey])
                  else:
                      s.op('act', lambda e: e.activation(out=dst, in_=pT[:, 0:rows], func=func), r=['pT'], w=[dstkey])

              if start <= 1:
                  load_vecT(cT[:], cvec[:, :], 32, func=AF.Silu, dstkey='cT')
              load_vecT(badT[:, 0:96], b_ada[0:96, :], 96, dstkey='badT')
              load_vecT(badT[:, 96:192], b_ada[96:192, :], 96, dstkey='badT')
              load_vecT(ln1gT[:], ln1_g[:, :], 32, dstkey='ln1gT')
              load_vecT(ln1bT[:], ln1_b[:, :], 32, dstkey='ln1bT')
              load_vecT(ln2gT[:], ln2_g[:, :], 32, dstkey='ln2gT')
              load_vecT(ln2bT[:], ln2_b[:, :], 32, dstkey='ln2bT')
              load_vecT(bgluT[:], b_glu[:, :], 16, dstkey='bgluT')
              load_vecT(dT[:], ssm_d[:, :], 16, dstkey='dT')

              if start <= 1:
                  wav = w_ada.rearrange("(j p) m -> p j m", p=128)
                  mrflat = modrow.rearrange("a b -> (a b)")
                  for n in range(96):
                      wt = wa[n % 2]
                      k = 'wa%d' % (n % 2)
                      s.dma('sp' if n % 2 == 0 else 'act', wt[:], wav[:, :, n * 256:(n + 1) * 256], w=[k])
                      for j in range(32):
                          s.op('pe', lambda e: e.matmul(pm[:], lhsT=cT[:, j:j + 1], rhs=wt[:, j, :],
                                                        start=(j == 0), stop=(j == 31)),
                               r=[k, 'cT'], w=['pm'], sig=(j == 31))
                      s.op('dve', lambda e: e.tensor_copy(out=mrow[:], in_=pm[:]), r=['pm'], w=['mrow'])
                      s.dma('sp', mrflat[n * 256:(n + 1) * 256].unsqueeze(0), mrow[:], r=['mrow'], w=['modrow'])
              load_vecT(modT[:, 0:96], modrow[0:96, :], 96, dstkey='modT')
              load_vecT(modT[:, 96:192], modrow[96:192, :], 96, dstkey='modT')
              s.op('dve', lambda e: e.tensor_tensor(out=modT[:], in0=modT[:], in1=badT[:], op=ALU.add),
                   r=['modT', 'badT'], w=['modT'])
              for dst, q, key in ((sc1p, 1, 'sc1p'), (g1p, 2, 'g1p'), (sc2p, 4, 'sc2p'), (g2p, 5, 'g2p')):
                  s.op('dve', lambda e: e.tensor_scalar(out=dst[:], in0=modT[:, q * 32:(q + 1) * 32], scalar1=1.0,
                                                        scalar2=None, op0=ALU.add), r=['modT'], w=[key])
              if debug:
                  s.dma('sp', dbg_modT, modT[:], r=['modT'], w=['dbg_modT'])
              s.barrier()
              stop_after(1)
          sh1 = modT[:, 0:32]
          sh2 = modT[:, 96:128]

          for st in phase(2):
              def sb(name, shape, dt=F32):
                  return st.enter_context(nc.sbuf_tensor(name, shape, dt))

              def ps(name, shape, dt=F32):
                  return st.enter_context(nc.psum_tensor(name, shape, dt))
              hT = sb("p2_hT", [128, 32, 512], F32R)
              xs = [sb("p2_xs%d" % i, [128, D]) for i in range(2)]
              wt2 = [sb("p2_w%d" % i, [128, 32, 256], F32R) for i in range(2)]
              ost = [sb("p2_o%d" % i, [128, 512], F32R) for i in range(2)]
              ptr = [ps("p2_ptr%d" % i, [128, 128]) for i in range(3)]
              pacc = [ps("p2_acc%d" % i, [128, 512]) for i in range(2)]
              wv = w_in.rearrange("(j p) m -> p j m", p=128)
              tcount = 0
              ocount = 0
              for tc in range(4):
                  for tt in range(4):
                      xb = xs[tt % 2]
                      xk = 'xs%d' % (tt % 2)
                      t0 = tc * 512 + tt * 128
                      s.dma('sp', xb[:], x[t0:t0 + 128, :], w=[xk])
                      for j in range(32):
                          pt = ptr[tcount % 3]
                          pk = 'ptr%d' % (tcount % 3)
                          tcount += 1
                          s.op('pe', lambda e: e.transpose(pt[:], xb[:, j * 128:(j + 1) * 128], ident[:]),
                               r=[xk, 'ident'], w=[pk])
                          dst = hT[:, j, tt * 128:(tt + 1) * 128]
                          if j % 2 == 0:
                              s.op('act', lambda e: e.activation(out=dst, in_=pt[:], func=AF.Identity,
                                                                 bias=sh1[:, j:j + 1], scale=sc1p[:, j:j + 1]),
                                   r=[pk, 'modT', 'sc1p'], w=['hT'])
                          else:
                              s.op('dve', lambda e: e.tensor_scalar(out=dst, in0=pt[:], scalar1=sc1p[:, j:j + 1],
                                                                    scalar2=sh1[:, j:j + 1], op0=ALU.mult, op1=ALU.add),
                                   r=[pk, 'modT', 'sc1p'], w=['hT'])
                  for mp in range(20):
                      wt = wt2[mp % 2]
                      wk = 'w2_%d' % (mp % 2)
                      s.dma('sp' if mp % 2 == 0 else 'act', wt[:], wv[:, :, mp * 256:(mp + 1) * 256], w=[wk])
                      for mh in range(2):
                          pa = pacc[mh]
                          pk = 'acc%d' % mh
                          for j in range(32):
                              s.op('pe', lambda e: e.matmul(pa[:], lhsT=wt[:, j, mh * 128:(mh + 1) * 128], rhs=hT[:, j, :],
                                                            start=(j == 0), stop=(j == 31)),
                                   r=[wk, 'hT'], w=[pk], sig=(j == 31))
                          ob = ost[ocount % 2]
                          ok = 'ost%d' % (ocount % 2)
                          ocount += 1
                          copy_on(evac_eng(), ob[:], pa[:], [pk], [ok])
                          m = mp * 2 + mh
                          s.dma('pool', projT[m * 128:(m + 1) * 128, tc * 512:(tc + 1) * 512], ob[:], r=[ok], w=['projT'])
              s.barrier()
              stop_after(2)

          for st in phase(3):
              def sb(name, shape, dt=F32):
                  return st.enter_context(nc.sbuf_tensor(name, shape, dt))

              def ps(name, shape, dt=F32):
                  return st.enter_context(nc.psum_tensor(name, shape, dt))
              pT3 = ps("p3_pT", [128, 128])
              pkb = ps("p3_pkb", [128, 128])
              pst = [ps("p3_pst%d" % i, [128, 256]) for i in range(2)]
              py3 = [ps("p3_py%d" % i, [128, 256]) for i in range(2)]
              stg3 = sb("p3_stg", [128, 2, 64])
              lr = sb("p3_lr", [128, 128]); li = sb("p3_li", [128, 128]); dtb = sb("p3_dt", [128, 128])
              t1 = sb("p3_t1", [128, 128]); t2 = sb("p3_t2", [128, 128]); t3 = sb("p3_t3", [128, 128])
              ki = sb("p3_ki", [128, 128], I32)
              lbr = sb("p3_lbr", [128, 128]); lbi = sb("p3_lbi", [128, 128])
              cr = sb("p3_cr", [128, 128]); ci = sb("p3_ci", [128, 128])
              Akr = sb("p3_Akr", [128, 9, 128]); Aki = sb("p3_Aki", [128, 9, 128])
              PWr = sb("p3_PWr", [128, 8, 128]); PWi = sb("p3_PWi", [128, 8, 128])
              PHr = sb("p3_PHr", [128, 8, 128]); PHi = sb("p3_PHi", [128, 8, 128])

              def vop(eng, out_ap, a, b, op, r, w):
                  s.op(eng, lambda e: e.tensor_tensor(out=out_ap, in0=a, in1=b, op=op), r=r, w=w)

              def load_pg(dst, src, key):
                  s.dma('sp', stg3[:], src.rearrange("d g p -> g d p"), w=['stg3'])
                  s.op('pe', lambda e: e.transpose(pT3[:], stg3[:].rearrange("g d p -> g (d p)"), ident[:]),
                       r=['stg3', 'ident'], w=['pT3'])
                  s.op('dve', lambda e: e.tensor_copy(out=dst[:], in_=pT3[:]), r=['pT3'], w=[key])
              load_pg(lr, lam_re, 'lr')
              load_pg(li, lam_im, 'li')
              for d in range(2):
                  s.dma('sp', dtb[d * 64:(d + 1) * 64, :], log_dt[d, :].partition_broadcast(64), w=['dtb'])
              s.op('act', lambda e: e.activation(out=dtb[:], in_=dtb[:], func=AF.Exp), r=['dtb'], w=['dtb'])
              sub_stop(1)
              vop('dve', t1[:], lr[:], dtb[:], ALU.mult, ['lr', 'dtb'], ['t1'])
              s.op('act', lambda e: e.activation(out=t1[:], in_=t1[:], func=AF.Exp), r=['t1'], w=['t1'])
              vop('dve', t2[:], li[:], dtb[:], ALU.mult, ['li', 'dtb'], ['t2'])

              def sin_of(dst, shift, key):
                  s.op('dve', lambda e: e.tensor_scalar(out=t3[:], in0=t2[:], scalar1=1.0 / TWO_PI,
                                                        scalar2=shift / TWO_PI + 0.5, op0=ALU.mult, op1=ALU.add),
                       r=['t2'], w=['t3'])
                  s.op('dve', lambda e: e.tensor_copy(out=ki[:], in_=t3[:]), r=['t3'], w=['ki'])
                  s.op('dve', lambda e: e.tensor_copy(out=dst[:], in_=ki[:]), r=['ki'], w=[key])
                  s.op('dve', lambda e: e.tensor_tensor(out=t3[:], in0=dst[:], in1=t3[:], op=ALU.is_gt),
                       r=[key, 't3'], w=['t3'])
                  vop('dve', dst[:], dst[:], t3[:], ALU.subtract, [key, 't3'], [key])
                  s.op('dve', lambda e: e.scalar_tensor_tensor(out=dst[:], in0=dst[:], scalar=-TWO_PI, in1=t2[:],
                                                               op0=ALU.mult, op1=ALU.add), r=[key, 't2'], w=[key])
                  s.op('dve', lambda e: e.tensor_scalar(out=dst[:], in0=dst[:], scalar1=float(shift), scalar2=math.pi,
                                                        op0=ALU.add, op1=ALU.min), r=[key], w=[key])
                  s.op('dve', lambda e: e.tensor_scalar(out=dst[:], in0=dst[:], scalar1=-math.pi, scalar2=None,
                                                        op0=ALU.max), r=[key], w=[key])
                  s.op('act', lambda e: e.activation(out=dst[:], in_=dst[:], func=AF.Sin), r=[key], w=[key])
              sin_of(lbi, 0.0, 'lbi')
              sub_stop(2)
              sin_of(lbr, math.pi / 2, 'lbr')
              vop('dve', lbr[:], lbr[:], t1[:], ALU.mult, ['lbr', 't1'], ['lbr'])
              vop('dve', lbi[:], lbi[:], t1[:], ALU.mult, ['lbi', 't1'], ['lbi'])
              if debug:
                  s.dma('sp', dbg_lb[:, 0:128], lbr[:], r=['lbr'], w=['dbg_lb'])
                  s.dma('sp', dbg_lb[:, 128:256], lbi[:], r=['lbi'], w=['dbg_lb'])
              vop('dve', t1[:], lr[:], lr[:], ALU.mult, ['lr'], ['t1'])
              vop('dve', t2[:], li[:], li[:], ALU.mult, ['li'], ['t2'])
              vop('dve', t1[:], t1[:], t2[:], ALU.add, ['t1', 't2'], ['t1'])
              s.op('dve', lambda e: e.reciprocal(out=t1[:], in_=t1[:]), r=['t1'], w=['t1'])
              s.op('dve', lambda e: e.tensor_scalar(out=t2[:], in0=lbr[:], scalar1=-1.0, scalar2=None, op0=ALU.add),
                   r=['lbr'], w=['t2'])
              vop('dve', cr[:], t2[:], lr[:], ALU.mult, ['t2', 'lr'], ['cr'])
              vop('dve', t3[:], lbi[:], li[:], ALU.mult, ['lbi', 'li'], ['t3'])
              vop('dve', cr[:], cr[:], t3[:], ALU.add, ['cr', 't3'], ['cr'])
              vop('dve', cr[:], cr[:], t1[:], ALU.mult, ['cr', 't1'], ['cr'])
              vop('dve', ci[:], lbi[:], lr[:], ALU.mult, ['lbi', 'lr'], ['ci'])
              vop('dve', t3[:], t2[:], li[:], ALU.mult, ['t2', 'li'], ['t3'])
              vop('dve', ci[:], ci[:], t3[:], ALU.subtract, ['ci', 't3'], ['ci'])
              vop('dve', ci[:], ci[:], t1[:], ALU.mult, ['ci', 't1'], ['ci'])
              sub_stop(3)
              s.op('dve', lambda e: e.memset(Akr[:, 0, :], 1.0), w=['Akr'])
              s.op('dve', lambda e: e.memset(Aki[:, 0, :], 0.0), w=['Aki'])
              for k in range(1, 9):
                  vop('dve', t1[:], Akr[:, k - 1, :], lbr[:], ALU.mult, ['Akr', 'lbr'], ['t1'])
                  vop('dve', t2[:], Aki[:, k - 1, :], lbi[:], ALU.mult, ['Aki', 'lbi'], ['t2'])
                  vop('dve', Akr[:, k, :], t1[:], t2[:], ALU.subtract, ['t1', 't2'], ['Akr'])
                  vop('dve', t1[:], Akr[:, k - 1, :], lbi[:], ALU.mult, ['Akr', 'lbi'], ['t1'])
                  vop('dve', t2[:], Aki[:, k - 1, :], lbr[:], ALU.mult, ['Aki', 'lbr'], ['t2'])
                  vop('dve', Aki[:, k, :], t1[:], t2[:], ALU.add, ['t1', 't2'], ['Aki'])
              sub_stop(32)
              for j in range(8):
                  for (dstp, srcp, kd, ks) in ((PWr, Akr, 'PWr', 'Akr'), (PWi, Aki, 'PWi', 'Aki'),
                                               (PHr, Akr, 'PHr', 'Akr'), (PHi, Aki, 'PHi', 'Aki')):
                      if kd.startswith('PW'):
                          ef, eb = 7 - j, j
                      else:
                          ef, eb = j + 1, 8 - j
                      s.op('dve', lambda e: e.tensor_copy(out=dstp[0:64, j, :], in_=srcp[0:64, ef, :]), r=[ks], w=[kd])
                      s.op('dve', lambda e: e.tensor_copy(out=dstp[64:128, j, :], in_=srcp[64:128, eb, :]), r=[ks], w=[kd])

              sub_stop(4)
              mkf = sb("p3_mkf", [128, 2])
              s.op('pool', lambda e: e.memset(mkf[:, :], 0.0), w=['mkf'])
              s.op('pool', lambda e: e.memset(mkf[0:64, 0:1], 1.0), w=['mkf'])
              s.op('pool', lambda e: e.memset(mkf[64:128, 1:2], 1.0), w=['mkf'])
              Cmk = [sb("p3_Cmk%d" % i, [128, 128]) for i in range(4)]
              Zr = sb("p3_Zr", [128, 16, 257]); Zi = sb("p3_Zi", [128, 16, 257])
              ZRr = sb("p3_ZRr", [128, 16, 257], F32R); ZRi = sb("p3_ZRi", [128, 16, 257], F32R)
              ta = [sb("p3_ta%d" % i, [128, 16]) for i in range(8)]
              Hs = [[sb("p3_H%d_%d" % (c_, x_), [128, 8, 128]) for x_ in range(2)] for c_ in range(2)]
              KBs = [sb("p3_KB%d" % c_, [128, 15, 128], F32R) for c_ in range(2)]
              Bre = sb("p3_Bre", [128, 8, 16]); Bim = sb("p3_Bim", [128, 8, 16])
              Bbr = sb("p3_Bbr", [128, 8, 16]); Bbi = sb("p3_Bbi", [128, 8, 16])
              tb1 = sb("p3_tb1", [128, 8, 16]); tb2 = sb("p3_tb2", [128, 8, 16])
              Cst = sb("p3_Cst", [128, 2, 64])
              Cre = sb("p3_Cre", [128, 128]); Cim = sb("p3_Cim", [128, 128]); nCim = sb("p3_nCim", [128, 128])
              ABr = sb("p3_ABr", [128, 8, 128]); ABi = sb("p3_ABi", [128, 8, 128])
              tab1 = sb("p3_tab1", [128, 8, 128]); tab2 = sb("p3_tab2", [128, 8, 128])
              ABT = sb("p3_ABT", [128, 8, 2, 128])
              GTg = sb("p3_GTg", [128, 8, 2, 128], F32R)
              HM = GTg
              uT = [sb("p3_uT%d" % c_, [128, T], F32R) for c_ in range(2)]
              yT = sb("p3_yT", [128, T])

              for gb in range(8):
                  G0 = gb * 16
                  s.op('dve', lambda e: e.memset(Zr[:, :, :], 0.0), w=['Zrf', 'Zrb'])
                  s.op('pool', lambda e: e.memset(Zi[:, :, :], 0.0), w=['Zif', 'Zib'])
                  for cl in range(2):
                      ct = gb * 2 + cl
                      g0 = ct * 8
                      uk = 'uT%d' % cl
                      s.dma('sp', uT[cl][:], projT[ct * 128:(ct + 1) * 128, :], r=['projT'], w=[uk])
                      for d in range(2):
                          s.dma('act', Bre[d * 64:(d + 1) * 64, :, :], b_re[d, g0:g0 + 8, :, :].rearrange("g p h -> p g h"), w=['Bre'])
                          s.dma('act', Bim[d * 64:(d + 1) * 64, :, :], b_im[d, g0:g0 + 8, :, :].rearrange("g p h -> p g h"), w=['Bim'])
                      for (src, dst, key) in ((c_re, Cre, 'Cre'), (c_im, Cim, 'Cim')):
                          s.dma('sp', Cst[:], src[:, g0:g0 + 8, :, :].rearrange("d g h p -> (g h) d p"), w=['Cst'])
                          s.op('pe', lambda e: e.transpose(pT3[:], Cst[:].rearrange("q d p -> q (d p)"), ident[:]),
                               r=['Cst', 'ident'], w=['pT3'])
                          s.op('dve', lambda e: e.tensor_copy(out=dst[:], in_=pT3[:]), r=['pT3'], w=[key])
                      s.op('dve', lambda e: e.tensor_scalar(out=nCim[:], in0=Cim[:], scalar1=-1.0, scalar2=None, op0=ALU.mult),
                           r=['Cim'], w=['nCim'])
                      crb = cr[:, g0:g0 + 8].unsqueeze(2).to_broadcast([128, 8, 16])
                      cib = ci[:, g0:g0 + 8].unsqueeze(2).to_broadcast([128, 8, 16])
                      vop('dve', tb1[:], Bre[:], crb, ALU.mult, ['Bre', 'cr'], ['tb1'])
                      vop('dve', tb2[:], Bim[:], cib, ALU.mult, ['Bim', 'ci'], ['tb2'])
                      vop('dve', Bbr[:], tb1[:], tb2[:], ALU.subtract, ['tb1', 'tb2'], ['Bbr'])
                      vop('dve', tb1[:], Bim[:], crb, ALU.mult, ['Bim', 'cr'], ['tb1'])
                      vop('dve', tb2[:], Bre[:], cib, ALU.mult, ['Bre', 'ci'], ['tb2'])
                      vop('dve', Bbi[:], tb1[:], tb2[:], ALU.add, ['tb1', 'tb2'], ['Bbi'])
                      pwr = PWr[:, :, g0:g0 + 8].unsqueeze(3).to_broadcast([128, 8, 8, 16])
                      pwi = PWi[:, :, g0:g0 + 8].unsqueeze(3).to_broadcast([128, 8, 8, 16])
                      bbr = Bbr[:].unsqueeze(1).to_broadcast([128, 8, 8, 16])
                      bbi = Bbi[:].unsqueeze(1).to_broadcast([128, 8, 8, 16])
                      v4 = lambda tl: tl[:].rearrange("p j (g h) -> p j g h", h=16)
                      vop('dve', v4(tab1), pwr, bbr, ALU.mult, ['PWr', 'Bbr'], ['tab1'])
                      vop('pool', v4(tab2), pwi, bbi, ALU.mult, ['PWi', 'Bbi'], ['tab2'])
                      vop('dve', ABr[:], tab1[:], tab2[:], ALU.subtract, ['tab1', 'tab2'], ['ABr'])
                      vop('dve', v4(tab1), pwr, bbi, ALU.mult, ['PWr', 'Bbi'], ['tab1'])
                      vop('pool', v4(tab2), pwi, bbr, ALU.mult, ['PWi', 'Bbr'], ['tab2'])
                      vop('dve', ABi[:], tab1[:], tab2[:], ALU.add, ['tab1', 'tab2'], ['ABi'])
                      phr = PHr[:, :, g0:g0 + 8].unsqueeze(3).to_broadcast([128, 8, 8, 16])
                      phi = PHi[:, :, g0:g0 + 8].unsqueeze(3).to_broadcast([128, 8, 8, 16])
                      creb = Cre[:].rearrange("p (g h) -> p g h", h=16).unsqueeze(1).to_broadcast([128, 8, 8, 16])
                      cimb = Cim[:].rearrange("p (g h) -> p g h", h=16).unsqueeze(1).to_broadcast([128, 8, 8, 16])
                      ncimb = nCim[:].rearrange("p (g h) -> p g h", h=16).unsqueeze(1).to_broadcast([128, 8, 8, 16])
                      HR, HI = Hs[cl]
                      hk = 'H%d' % cl
                      vop('dve', v4(tab1), creb, phr, ALU.mult, ['Cre', 'PHr'], ['tab1'])
                      vop('pool', v4(tab2), cimb, phi, ALU.mult, ['Cim', 'PHi'], ['tab2'])
                      vop('dve', HR[:], tab1[:], tab2[:], ALU.subtract, ['tab1', 'tab2'], [hk])
                      vop('dve', v4(tab1), creb, phi, ALU.mult, ['Cre', 'PHi'], ['tab1'])
                      vop('pool', v4(tab2), ncimb, phr, ALU.mult, ['nCim', 'PHr'], ['tab2'])
                      vop('dve', HI[:], tab2[:], tab1[:], ALU.subtract, ['tab1', 'tab2'], [hk])
                      sub_stop(5)
                      KB = KBs[cl]
                      kk = 'KB%d' % cl
                      for ci_, (csrc, ck) in enumerate(((Cre, 'Cre'), (nCim, 'nCim'))):
                          for di_ in range(2):
                              s.op('dve' if di_ else 'pool',
                                   lambda e: e.tensor_scalar(out=Cmk[ci_ * 2 + di_][:], in0=csrc[:], scalar1=mkf[:, di_:di_ + 1],
                                                             scalar2=None, op0=ALU.mult), r=[ck, 'mkf'], w=['Cmk'])
                      for dl in range(-7, 8):
                          terms = []
                          if dl >= 0:
                              terms.append((0, 7 - dl))
                          if dl <= 0:
                              terms.append((1, -dl))
                          n = 0
                          for (di_, jj) in terms:
                              for (ab, ci_, ka) in ((ABr, 0, 'ABr'), (ABi, 1, 'ABi')):
                                  last = (n == 2 * len(terms) - 1)
                                  s.op('pe', lambda e: e.matmul(pkb[:], lhsT=ab[:, jj, :], rhs=Cmk[ci_ * 2 + di_][:],
                                                                start=(n == 0), stop=last),
                                       r=[ka, 'Cmk'], w=['pkb'], sig=last)
                                  n += 1
                          s.op('dve', lambda e: e.tensor_tensor(out=KB[:, dl + 7, :], in0=pkb[:], in1=blockmask[:], op=ALU.mult),
                               r=['pkb', 'blockmask'], w=[kk])
                      s.op('dve', lambda e: e.scalar_tensor_tensor(out=KB[:, 7, :], in0=ident[:], scalar=dT[:, ct:ct + 1],
                                                                   in1=KB[:, 7, :], op0=ALU.mult, op1=ALU.add),
                           r=[kk, 'ident', 'dT'], w=[kk])
                      sub_stop(6)
                      for j in range(8):
                          for ri, (ab, ka) in enumerate(((ABr, 'ABr'), (ABi, 'ABi'))):
                              s.op('pe', lambda e: e.transpose(pT3[:], ab[:, j, :], ident[:]), r=[ka, 'ident'], w=['pT3'])
                              copy_on(evac_eng(), ABT[:, j, ri, :], pT3[:], ['pT3'], ['ABT'])
                      sub_stop(7)
                      for g in range(8):
                          gl = cl * 8 + g
                          s.op('pool', lambda e: e.tensor_scalar(out=GTg[:].rearrange("p j r m -> p (j r m)"),
                                                                 in0=ABT[:].rearrange("p j r m -> p (j r m)"),
                                                                 scalar1=rowmask[:, g:g + 1], scalar2=None, op0=ALU.mult),
                               r=['ABT', 'rowmask'], w=['GTg'])
                          for ri, (Z, zk) in enumerate(((Zr, 'Zr'), (Zi, 'Zi'))):
                              pp = pst[ri]
                              pk = 'pst%d' % ri
                              for j in range(8):
                                  s.op('pe', lambda e: e.matmul(pp[:], lhsT=GTg[:, j, ri, :],
                                                                rhs=uT[cl][:].rearrange("p (c j) -> p j c", j=8)[:, j, :],
                                                                start=(j == 0), stop=(j == 7)),
                                       r=['GTg', uk], w=[pk], sig=(j == 7))
                              copy_on('act', Z[0:64, gl, 1:257], pp[0:64, :], [pk], [zk + 'f'])
                              copy_on('dve', Z[64:128, gl, 0:256], pp[64:128, :], [pk], [zk + 'b'])
                  sub_stop(8)
                  a8r = Akr[:, 8, G0:G0 + 16]
                  a8i = Aki[:, 8, G0:G0 + 16]
                  for c_ in range(255):
                      for (eng, psl, o, tt_) in (('dve', slice(0, 64), 'f', ta[0:4]), ('pool', slice(64, 128), 'b', ta[4:8])):
                          if o == 'f':
                              src, dst = c_, c_ + 1
                          else:
                              src, dst = 256 - c_, 255 - c_
                          m1, m2, m3, m4 = tt_
                          kr, ki_ = 'Zr' + o, 'Zi' + o
                          vop(eng, m1[psl, :], Zr[psl, :, src], a8r[psl, :], ALU.mult, [kr, 'Akr'], ['m1' + o])
                          vop(eng, m2[psl, :], Zi[psl, :, src], a8i[psl, :], ALU.mult, [ki_, 'Aki'], ['m2' + o])
                          vop(eng, m3[psl, :], Zr[psl, :, src], a8i[psl, :], ALU.mult, [kr, 'Aki'], ['m3' + o])
                          vop(eng, m4[psl, :], Zi[psl, :, src], a8r[psl, :], ALU.mult, [ki_, 'Akr'], ['m4' + o])
                          vop(eng, m1[psl, :], m1[psl, :], m2[psl, :], ALU.subtract, ['m1' + o, 'm2' + o], ['m1' + o])
                          vop(eng, m3[psl, :], m3[psl, :], m4[psl, :], ALU.add, ['m3' + o, 'm4' + o], ['m3' + o])
                          vop(eng, Zr[psl, :, dst], Zr[psl, :, dst], m1[psl, :], ALU.add, [kr, 'm1' + o], [kr])
                          vop(eng, Zi[psl, :, dst], Zi[psl, :, dst], m3[psl, :], ALU.add, [ki_, 'm3' + o], [ki_])
                  sub_stop(9)
                  ZrR = ZRr[:]
                  ZiR = ZRi[:]
                  s.op('dve', lambda e: e.tensor_copy(out=ZRr[0:64, :, 0:256], in_=Zr[0:64, :, 0:256]), r=['Zrf'], w=['ZRr'])
                  s.op('pool', lambda e: e.tensor_copy(out=ZRr[64:128, :, 0:256], in_=Zr[64:128, :, 1:257]), r=['Zrb'], w=['ZRr'])
                  s.op('dve', lambda e: e.tensor_copy(out=ZRi[0:64, :, 0:256], in_=Zi[0:64, :, 0:256]), r=['Zif'], w=['ZRi'])
                  s.op('pool', lambda e: e.tensor_copy(out=ZRi[64:128, :, 0:256], in_=Zi[64:128, :, 1:257]), r=['Zib'], w=['ZRi'])
                  for cl in range(2):
                      ct = gb * 2 + cl
                      HR, HI = Hs[cl]
                      hk = 'H%d' % cl
                      KB = KBs[cl]
                      kk = 'KB%d' % cl
                      uk = 'uT%d' % cl
                      for i in range(8):
                          for xi, Hx in enumerate((HR, HI)):
                              s.op('pool' if xi else 'dve',
                                   lambda e: e.tensor_tensor(out=HM[:, :, xi, :], in0=colmask[:],
                                                             in1=Hx[:, i, :].unsqueeze(1).to_broadcast([128, 8, 128]), op=ALU.mult),
                                   r=[hk, 'colmask'], w=['GTg'])
                          pp = py3[i % 2]
                          pk = 'py%d' % (i % 2)
                          nmm = 8 + 16
                          n = 0
                          for j in range(8):
                              s.op('pe', lambda e: e.matmul(pp[:], lhsT=KB[:, (i - j) + 7, :],
                                                            rhs=uT[cl][:].rearrange("p (c j) -> p j c", j=8)[:, j, :],
                                                            start=(n == 0), stop=False),
                                   r=[kk, uk], w=[pk], sig=False)
                              n += 1
                          for g in range(8):
                              gl = cl * 8 + g
                              for xi, ZR in enumerate((ZrR, ZiR)):
                                  last = (n == nmm - 1)
                                  s.op('pe', lambda e: e.matmul(pp[:], lhsT=HM[:, g, xi, :], rhs=ZR[:, gl, 0:256],
                                                                start=False, stop=last),
                                       r=['GTg', 'ZRr', 'ZRi'], w=[pk], sig=last)
                                  n += 1
                          copy_on(evac_eng(), yT[:].rearrange("p (c j) -> p j c", j=8)[:, i, :], pp[:], [pk], ['yT'])
                      s.dma('sp', ymixT[ct * 128:(ct + 1) * 128, :], yT[:], r=['yT'], w=['ymixT'])
              s.barrier()
              stop_after(3)

          for st in phase(4):
              def sb(name, shape, dt=F32):
                  return st.enter_context(nc.sbuf_tensor(name, shape, dt))

              def ps(name, shape, dt=F32):
                  return st.enter_context(nc.psum_tensor(name, shape, dt))
              bias = sb("p4_bias", [128, 3, 16, 128])
              dq = sb("p4_dq", [128, 128])
              es = sb("p4_es", [128, 16])
              kT = sb("p4_kT", [128, T], F32R)
              vTt = sb("p4_vT", [128, T])
              V = sb("p4_V", [128, 16, 128], F32R)
              qT = sb("p4_qT", [128, 4, T], F32R)
              tmp = [sb("p4_tmp%d" % i, [128, 512]) for i in range(2)]
              pTt = [sb("p4_pT%d" % i, [128, 512], F32R) for i in range(2)]
              den = sb("p4_den", [128, 512])
              yo = [sb("p4_yo%d" % i, [128, 512]) for i in range(2)]
              ptv = ps("p4_ptv", [128, 128])
              psT = [ps("p4_psT%d" % i, [128, 512]) for i in range(3)]
              po = ps("p4_po", [128, 512])
              prs = ps("p4_prs", [128, 512])
              s.op('pool', lambda e: e.iota(dq[:], pattern=[[1, 128]], base=0, channel_multiplier=-1,
                                            allow_small_or_imprecise_dtypes=True), w=['dq'])
              adq = sb("p4_adq", [128, 128])
              s.op('dve', lambda e: e.tensor_scalar(out=adq[:], in0=dq[:], scalar1=-1.0, scalar2=None, op0=ALU.mult), r=['dq'], w=['adq'])
              s.op('dve', lambda e: e.tensor_tensor(out=adq[:], in0=adq[:], in1=dq[:], op=ALU.max), r=['dq', 'adq'], w=['adq'])
              for h in range(16):
                  slope = 2.0 ** (-8.0 * (h + 1) / 16.0)
                  s.op('dve', lambda e: e.tensor_scalar(out=bias[:, 0, h, :], in0=dq[:], scalar1=128.0, scalar2=-slope,
                                                        op0=ALU.add, op1=ALU.mult), r=['dq'], w=['bias'])
                  s.op('pool', lambda e: e.affine_select(out=bias[:, 0, h, :], in_=bias[:, 0, h, :], pattern=[[-1, 128]],
                                                         compare_op=ALU.is_ge, fill=-1e30, base=0, channel_multiplier=1),
                       r=['bias'], w=['bias'])
                  s.op('dve', lambda e: e.tensor_scalar(out=bias[:, 1, h, :], in0=adq[:], scalar1=-slope, scalar2=None,
                                                        op0=ALU.mult), r=['adq'], w=['bias'])
                  s.op('dve', lambda e: e.tensor_scalar(out=bias[:, 2, h, :], in0=dq[:], scalar1=-128.0, scalar2=slope,
                                                        op0=ALU.add, op1=ALU.mult), r=['dq'], w=['bias'])
                  s.op('pool', lambda e: e.affine_select(out=bias[:, 2, h, :], in_=bias[:, 2, h, :], pattern=[[1, 128]],
                                                         compare_op=ALU.is_ge, fill=-1e30, base=0, channel_multiplier=-1),
                       r=['bias'], w=['bias'])
              s.dma('sp', es[:], sink[0, :].partition_broadcast(128), w=['es'])
              s.op('act', lambda e: e.activation(out=es[:], in_=es[:], func=AF.Exp), r=['es'], w=['es'])
              scale = 128.0 ** -0.5
              it = 0
              for kh in range(4):
                  s.dma('sp', kT[:], projT[(32 + kh) * 128:(33 + kh) * 128, :], r=['projT'], w=['kT'])
                  s.dma('act', vTt[:], projT[(36 + kh) * 128:(37 + kh) * 128, :].bitcast(F32), r=['projT'], w=['vTt'])
                  for hh in range(4):
                      m = 16 + kh * 4 + hh
                      s.dma('sp' if hh % 2 else 'act', qT[:, hh, :], projT[m * 128:(m + 1) * 128, :], r=['projT'], w=['qT'])
                  for kb in range(16):
                      s.op('pe', lambda e: e.transpose(ptv[:], vTt[:, kb * 128:(kb + 1) * 128], ident[:]),
                           r=['vTt', 'ident'], w=['ptv'])
                      copy_on(evac_eng(), V[:, kb, :], ptv[:], ['ptv'], ['V'])
                  for n in range(16):
                      rels = [r_ for r_ in (-1, 0, 1) if 0 <= n + r_ < 16]
                      for ri_, rel in enumerate(rels):
                          kb = n + rel
                          pS = psT[it % 3]
                          pk = 'psT%d' % (it % 3)
                          tm = tmp[it % 2]
                          tk = 'tmp%d' % (it % 2)
                          pt_ = pTt[it % 2]
                          ptk = 'pTt%d' % (it % 2)
                          it += 1
                          s.op('pe', lambda e: e.matmul(pS[:], lhsT=kT[:, kb * 128:(kb + 1) * 128],
                                                        rhs=qT[:, :, n * 128:(n + 1) * 128], start=True, stop=True),
                               r=['kT', 'qT'], w=[pk])
                          s.op('dve', lambda e: e.scalar_tensor_tensor(
                              out=tm[:].rearrange("p (h q) -> p h q", h=4), in0=pS[:].rearrange("p (h q) -> p h q", h=4),
                              scalar=scale, in1=bias[:, rel + 1, kh * 4:(kh + 1) * 4, :], op0=ALU.mult, op1=ALU.add),
                              r=[pk, 'bias'], w=[tk])
                          s.op('act', lambda e: e.activation(out=pt_[:], in_=tm[:], func=AF.Exp), r=[tk], w=[ptk])
                          first = (ri_ == 0)
                          last = (ri_ == len(rels) - 1)
                          s.op('pe', lambda e: e.matmul(po[:], lhsT=V[:, kb, :], rhs=pt_[:], start=first, stop=last),
                               r=['V', ptk], w=['po'])
                          s.op('pe', lambda e: e.matmul(prs[:], lhsT=onesR[:], rhs=pt_[:], start=first, stop=last),
                               r=['onesR', ptk], w=['prs'])
                      yb = yo[n % 2]
                      yk = 'yo%d' % (n % 2)
                      s.op('dve', lambda e: e.tensor_tensor(out=den[:].rearrange("p (h q) -> p h q", h=4),
                                                            in0=prs[:].rearrange("p (h q) -> p h q", h=4),
                                                            in1=es[:, kh * 4:(kh + 1) * 4].unsqueeze(2).to_broadcast([128, 4, 128]),
                                                            op=ALU.add), r=['prs', 'es'], w=['den'])
                      s.op('dve', lambda e: e.reciprocal(out=den[:], in_=den[:]), r=['den'], w=['den'])
                      s.op('dve', lambda e: e.tensor_tensor(out=yb[:], in0=po[:], in1=den[:], op=ALU.mult),
                           r=['po', 'den'], w=[yk])
                      s.dma('pool', ymixT[2048 + kh * 512:2048 + (kh + 1) * 512, n * 128:(n + 1) * 128].rearrange("(h p) q -> p h q", p=128),
                            yb[:].rearrange("p (h q) -> p h q", h=4), r=[yk], w=['ymixT'])
              s.barrier()
              stop_after(4)

          def layer_norm(st_sb, r_t, sq_t, rkey, sqkey, pstat, gT, bT, gk, bk):
              stat = st_sb
              s.op('pe', lambda e: None, r=[], w=[]) if False else None
              for j in range(32):
                  s.op('pe', lambda e: e.matmul(pstat[:], lhsT=onesM[:], rhs=r_t[:, j, :], start=(j == 0), stop=(j == 31)),
                       r=['onesM', rkey], w=['pstat'], sig=(j == 31))
              s.op('dve', lambda e: e.tensor_tensor(out=r_t[:], in0=r_t[:], in1=pstat[:].unsqueeze(1).to_broadcast([128, 32, 256]),
                                                    op=ALU.subtract), r=[rkey, 'pstat'], w=[rkey])
              s.op('pool', lambda e: e.tensor_tensor(out=sq_t[:], in0=r_t[:], in1=r_t[:], op=ALU.mult), r=[rkey], w=[sqkey])
              for j in range(32):
                  s.op('pe', lambda e: e.matmul(pstat[:], lhsT=onesM[:], rhs=sq_t[:, j, :], start=(j == 0), stop=(j == 31)),
                       r=['onesM', sqkey], w=['pstat'], sig=(j == 31))
              s.op('act', lambda e: e.activation(out=stat[:], in_=pstat[:], func=AF.Sqrt, bias=epsc[:, 0:1], scale=1.0),
                   r=['pstat', 'epsc'], w=['stat'])
              s.op('dve', lambda e: e.reciprocal(out=stat[:], in_=stat[:]), r=['stat'], w=['stat'])
              s.op('dve', lambda e: e.tensor_tensor(out=r_t[:], in0=r_t[:], in1=stat[:].unsqueeze(1).to_broadcast([128, 32, 256]),
                                                    op=ALU.mult), r=[rkey, 'stat'], w=[rkey])
              s.op('pool', lambda e: e.tensor_tensor(out=r_t[:], in0=r_t[:], in1=gT[:].unsqueeze(2).to_broadcast([128, 32, 256]),
                                                     op=ALU.mult), r=[rkey, gk], w=[rkey])
              s.op('dve', lambda e: e.tensor_tensor(out=r_t[:], in0=r_t[:], in1=bT[:].unsqueeze(2).to_broadcast([128, 32, 256]),
                                                    op=ALU.add), r=[rkey, bk], w=[rkey])

          epsc = gsb("epsc", [128, 1])
          s.op('pool', lambda e: e.memset(epsc[:], EPS), w=['epsc'])

          for st in phase(5):
              def sb(name, shape, dt=F32):
                  return st.enter_context(nc.sbuf_tensor(name, shape, dt))

              def ps(name, shape, dt=F32):
                  return st.enter_context(nc.psum_tensor(name, shape, dt))
              A = sb("p5_A", [128, 32, 256])
              B = sb("p5_B", [128, D])
              mixR = sb("p5_mixR", [128, 32, 256], F32R)
              R = sb("p5_R", [128, 32, 256])
              wo = [sb("p5_wo%d" % i, [128, 32, 128], F32R) for i in range(2)]
              wg = [sb("p5_wg%d" % i, [128, 16, 128], F32R) for i in range(2)]
              gt = sb("p5_gt", [128, 256])
              stat = sb("p5_stat", [128, 256])
              wr = sb("p5_wr", [128, 32, 72])
              brB = sb("p5_brB", [128, 72])
              lg = sb("p5_lg", [128, 72])
              sm = [sb("p5_sm%d" % i, [128, 8]) for i in range(6)]
              sc = [sb("p5_sc%d" % i, [128, 1]) for i in range(8)]
              e3 = sb("p5_e3", [128, 8, 8])
              pacc = [ps("p5_acc%d" % i, [128, 256]) for i in range(2)]
              pstat = ps("p5_pstat", [128, 256])
              ptr = [ps("p5_ptr%d" % i, [128, 128]) for i in range(2)]
              plg = ps("p5_plg", [128, 72])
              s.dma('sp', wr[:], w_r.rearrange("(j p) n -> p j n", p=128), w=['wr'])
              s.dma('sp', brB[:], b_r[0, :].partition_broadcast(128), w=['brB'])
              ymv = ymixT.rearrange("(c p) t -> p c t", p=128)
              wgv = w_glu.rearrange("(k p) m -> p k m", p=128)
              wov = w_out.rearrange("(k p) m -> p k m", p=128)
              x1v = x1T.rearrange("(j p) t -> p j t", p=128)
              gyT = sb("p5_gy", [128, 16, 256], F32R)
              gyR = gyT[:]
              gyF = B[:, 0:16 * 256].rearrange("p (k t) -> p k t", k=16)
              tcount = 0
              for tq in range(8):
                  tsl = slice(tq * 256, (tq + 1) * 256)
                  s.dma('sp', A[:], ymv[:, :, tsl], r=['ymixT'], w=['A'])
                  s.op('act', lambda e: e.activation(out=gyR, in_=A[:, 0:16, :], func=AF.Gelu), r=['A'], w=['gy'])
                  for m in range(16):
                      wt = wg[m % 2]
                      wk = 'wg%d' % (m % 2)
                      s.dma('act' if m % 2 else 'pool', wt[:], wgv[:, :, m * 128:(m + 1) * 128], w=[wk])
                      pa = pacc[m % 2]
                      pk = 'acc%d' % (m % 2)
                      for k in range(16):
                          s.op('pe', lambda e: e.matmul(pa[:], lhsT=wt[:, k, :], rhs=gyR[:, k, :], start=(k == 0), stop=(k == 15)),
                               r=[wk, 'gy'], w=[pk], sig=(k == 15))
                      s.op('act', lambda e: e.activation(out=gt[:], in_=pa[:], func=AF.Sigmoid, bias=bgluT[:, m:m + 1], scale=1.0),
                           r=[pk, 'bgluT'], w=['gt'])
                      s.op('dve', lambda e: e.tensor_tensor(out=mixR[:, m, :], in0=A[:, m, :], in1=gt[:], op=ALU.mult),
                           r=['A', 'gt'], w=['mixR'])
                  s.op('pool', lambda e: e.tensor_copy(out=mixR[:, 16:32, :], in_=A[:, 16:32, :]), r=['A'], w=['mixR'])
                  for tt in range(2):
                      t0 = tq * 256 + tt * 128
                      s.dma('sp', B[:], x[t0:t0 + 128, :], w=['B'])
                      for j in range(32):
                          pt = ptr[tcount % 2]
                          pk = 'ptr%d' % (tcount % 2)
                          tcount += 1
                          s.op('pe', lambda e: e.transpose(pt[:], B[:, j * 128:(j + 1) * 128], ident[:]), r=['B', 'ident'], w=[pk])
                          dst = R[:, j, tt * 128:(tt + 1) * 128]
                          if j % 2:
                              s.op('act', lambda e: e.mul(out=dst, in_=pt[:], mul=ALPHA) if False else
                                   e.activation(out=dst, in_=pt[:], func=AF.Identity, scale=ALPHA), r=[pk], w=['R'])
                          else:
                              s.op('dve', lambda e: e.tensor_scalar(out=dst, in0=pt[:], scalar1=ALPHA, scalar2=None, op0=ALU.mult),
                                   r=[pk], w=['R'])
                  for m in range(32):
                      wt = wo[m % 2]
                      wk = 'wo%d' % (m % 2)
                      s.dma('sp' if m % 2 else 'act', wt[:], wov[:, :, m * 128:(m + 1) * 128], w=[wk])
                      pa = pacc[m % 2]
                      pk = 'acc%d' % (m % 2)
                      for k in range(32):
                          s.op('pe', lambda e: e.matmul(pa[:], lhsT=wt[:, k, :], rhs=mixR[:, k, :], start=(k == 0), stop=(k == 31)),
                               r=[wk, 'mixR'], w=[pk], sig=(k == 31))
                      s.op('dve', lambda e: e.scalar_tensor_tensor(out=R[:, m, :], in0=pa[:], scalar=g1p[:, m:m + 1],
                                                                   in1=R[:, m, :], op0=ALU.mult, op1=ALU.add),
                           r=[pk, 'g1p', 'R'], w=['R'])
                  layer_norm(stat, R, A, 'R', 'A', pstat, ln1gT, ln1bT, 'ln1gT', 'ln1bT')
                  s.dma('sp', x1v[:, :, tsl], R[:], r=['R'], w=['x1T'])
                  s.op('dve', lambda e: e.tensor_tensor(out=A[:], in0=R[:], in1=sc2p[:].unsqueeze(2).to_broadcast([128, 32, 256]),
                                                        op=ALU.mult), r=['R', 'sc2p'], w=['A'])
                  s.op('pool', lambda e: e.tensor_tensor(out=A[:], in0=A[:], in1=sh2.unsqueeze(2).to_broadcast([128, 32, 256]),
                                                         op=ALU.add), r=['A', 'modT'], w=['A'])
                  for tt in range(2):
                      tile = tq * 2 + tt
                      t0 = tile * 128
                      for j in range(32):
                          pt = ptr[tcount % 2]
                          pk = 'ptr%d' % (tcount % 2)
                          tcount += 1
                          s.op('pe', lambda e: e.transpose(pt[:], A[:, j, tt * 128:(tt + 1) * 128], ident[:]), r=['A', 'ident'], w=[pk])
                          copy_on(evac_eng(), B[:].bitcast(F32R)[:, j * 128:(j + 1) * 128], pt[:], [pk], ['B'])
                      s.dma('sp', h2tm[t0:t0 + 128, :], B[:].bitcast(F32R), r=['B'], w=['h2tm'])
                      for j in range(32):
                          s.op('pe', lambda e: e.matmul(plg[:], lhsT=A[:, j, tt * 128:(tt + 1) * 128], rhs=wr[:, j, :],
                                                        start=(j == 0), stop=(j == 31)), r=['A', 'wr'], w=['plg'], sig=(j == 31))
                      s.op('dve', lambda e: e.tensor_tensor(out=lg[:], in0=plg[:], in1=brB[:], op=ALU.add), r=['plg', 'brB'], w=['lg'])
                      gmax, ngmax, gsum, m1, m2, dm, w1, w2 = sc
                      ge, sel, oh1, sel2, oh2, wtmp = sm
                      Gt = Gall[:, tile, :]
                      s.op('dve', lambda e: e.tensor_reduce(out=gmax[:], in_=lg[:, 0:8], axis=AX.X, op=ALU.max), r=['lg'], w=['rt'])
                      s.op('dve', lambda e: e.tensor_scalar(out=ngmax[:], in0=gmax[:], scalar1=-1.0, scalar2=None, op0=ALU.mult), r=['rt'], w=['rt'])
                      s.op('act', lambda e: e.activation(out=ge[:], in_=lg[:, 0:8], func=AF.Exp, bias=ngmax[:, 0:1], scale=1.0),
                           r=['lg', 'rt'], w=['rt2'])
                      s.op('dve', lambda e: e.tensor_reduce(out=gsum[:], in_=ge[:], axis=AX.X, op=ALU.add), r=['rt2'], w=['rt'])
                      s.op('dve', lambda e: e.reciprocal(out=gsum[:], in_=gsum[:]), r=['rt'], w=['rt'])
                      s.op('dve', lambda e: e.tensor_scalar(out=Gt, in0=lg[:, 0:8], scalar1=gmax[:, 0:1], scalar2=None, op0=ALU.is_equal),
                           r=['lg', 'rt'], w=['Gall'])
                      s.op('dve', lambda e: e.tensor_tensor(out=e3[:], in0=lg[:, 8:72].rearrange("p (g e) -> p g e", g=8),
                                                            in1=Gt.unsqueeze(2).to_broadcast([128, 8, 8]), op=ALU.mult),
                           r=['lg', 'Gall'], w=['e3'])
                      s.op('dve', lambda e: e.tensor_reduce(out=sel[:], in_=e3[:].rearrange("p g e -> p e g"), axis=AX.X, op=ALU.add),
                           r=['e3'], w=['rt'])
                      s.op('dve', lambda e: e.tensor_reduce(out=m1[:], in_=sel[:], axis=AX.X, op=ALU.max), r=['rt'], w=['rt'])
                      s.op('dve', lambda e: e.tensor_scalar(out=oh1[:], in0=sel[:], scalar1=m1[:, 0:1], scalar2=None, op0=ALU.is_equal), r=['rt'], w=['rt'])
                      s.op('dve', lambda e: e.scalar_tensor_tensor(out=sel2[:], in0=oh1[:], scalar=-1e30, in1=sel[:], op0=ALU.mult, op1=ALU.add), r=['rt'], w=['rt'])
                      s.op('dve', lambda e: e.tensor_reduce(out=m2[:], in_=sel2[:], axis=AX.X, op=ALU.max), r=['rt'], w=['rt'])
                      s.op('dve', lambda e: e.tensor_scalar(out=oh2[:], in0=sel2[:], scalar1=m2[:, 0:1], scalar2=None, op0=ALU.is_equal), r=['rt'], w=['rt'])
                      s.op('dve', lambda e: e.tensor_tensor(out=dm[:], in0=m1[:], in1=m2[:], op=ALU.subtract), r=['rt'], w=['rt'])
                      s.op('act', lambda e: e.activation(out=w1[:], in_=dm[:], func=AF.Sigmoid), r=['rt'], w=['rt3'])
                      s.op('dve', lambda e: e.tensor_scalar(out=w2[:], in0=w1[:], scalar1=-1.0, scalar2=1.0, op0=ALU.mult, op1=ALU.add), r=['rt3'], w=['rt'])
                      s.op('dve', lambda e: e.tensor_scalar(out=wtmp[:], in0=oh1[:], scalar1=w1[:, 0:1], scalar2=None, op0=ALU.mult), r=['rt', 'rt3'], w=['rt'])
                      s.op('dve', lambda e: e.scalar_tensor_tensor(out=wtmp[:], in0=oh2[:], scalar=w2[:, 0:1], in1=wtmp[:], op0=ALU.mult, op1=ALU.add), r=['rt'], w=['rt'])
                      s.op('dve', lambda e: e.tensor_scalar(out=WTall[:, tile, :], in0=wtmp[:], scalar1=gsum[:, 0:1], scalar2=None, op0=ALU.mult),
                           r=['rt'], w=['WTall'])
              if debug:
                  s.dma('sp', dbg_G, Gall[:].rearrange('p a b -> p (a b)'), r=['Gall'], w=['dbg_G'])
                  s.dma('sp', dbg_W, WTall[:].rearrange('p a b -> p (a b)'), r=['WTall'], w=['dbg_W'])
              s.barrier()
              stop_after(5)

          for st in phase(6):
              def sb(name, shape, dt=F32):
                  return st.enter_context(nc.sbuf_tensor(name, shape, dt))

              def ps(name, shape, dt=F32):
                  return st.enter_context(nc.psum_tensor(name, shape, dt))
              pb = [ps("p6_b%d" % i, [128, 512]) for i in range(8)]
              Pg = sb("p6_Pg", [128, 16, CAP], F32R)
              big = sb("p6_big", [128, 32 * CAP], F32R)
              hbT = sb("p6_hbT", [128, 32, CAP], F32R)
              WTB = sb("p6_WTB", [128, 8, CAP])
              wtT = sb("p6_wtT", [8, CAP])
              h2s = [sb("p6_h2s%d" % i, [128, 1024], F32R) for i in range(2)]
              wtl = [sb("p6_wt%d" % i, [128, 32, 128], F32R) for i in range(2)]
              sg = sb("p6_sg", [128, CAP])
              yst = [sb("p6_yst%d" % i, [128, 256], F32R) for i in range(2)]
              pp8 = sb("p6_pp8", [128, 8])
              for tt in range(16):
                  n = 0
                  for t2_ in range(tt + 1):
                      lh = onesF if t2_ < tt else ustrict
                      s.op('pe', lambda e: e.matmul(pb[0][:, 0:8], lhsT=lh[:], rhs=Gall[:, t2_, :], start=(n == 0), stop=(t2_ == tt)),
                           r=['Gall', 'onesF', 'ustrict'], w=['pb0'], sig=(t2_ == tt))
                      n += 1
                  s.op('dve', lambda e: e.tensor_tensor(out=pp8[:], in0=pb[0][:, 0:8], in1=Gall[:, tt, :], op=ALU.mult),
                       r=['pb0', 'Gall'], w=['pp8'])
                  s.op('dve', lambda e: e.tensor_reduce(out=posall[:, tt:tt + 1], in_=pp8[:], axis=AX.X, op=ALU.add),
                       r=['pp8'], w=['posall'])
              hTg = big[:, 0:32 * CAP].rearrange("p (j c) -> p j c", j=32)
              wd = big[:, 0:32 * 256].rearrange("p (k c) -> p k c", k=32)
              PgF = Pg[:].bitcast(F32)
              yav = yall
              wcount = 0
              ycount = 0
              for g in range(8):
                  for tt in range(16):
                      s.op('dve' if tt % 2 else 'pool',
                           lambda e: e.tensor_scalar(out=Pg[:, tt, :], in0=iotaS[:], scalar1=posall[:, tt:tt + 1],
                                                     scalar2=Gall[:, tt, g:g + 1], op0=ALU.is_equal, op1=ALU.mult),
                           r=['iotaS', 'posall', 'Gall'], w=['Pg'])
                  for jb in range(4):
                      for tt in range(16):
                          hb_ = h2s[tt % 2]
                          hk = 'h2s%d' % (tt % 2)
                          s.dma('sp' if tt % 2 else 'act', hb_[:], h2tm[tt * 128:(tt + 1) * 128, jb * 1024:(jb + 1) * 1024],
                                r=['h2tm'], w=[hk])
                          for jj in range(8):
                              s.op('pe', lambda e: e.matmul(pb[jj][:, 0:CAP], lhsT=hb_[:, jj * 128:(jj + 1) * 128], rhs=Pg[:, tt, :],
                                                            start=(tt == 0), stop=(tt == 15)),
                                   r=[hk, 'Pg'], w=['pb%d' % jj], sig=(jj == 7))
                      for jj in range(8):
                          copy_on(evac_eng(), hTg[:, jb * 8 + jj, :], pb[jj][:, 0:CAP], ['pb%d' % jj], ['big'])
                  for tt in range(16):
                      s.op('pe', lambda e: e.matmul(pb[0][0:8, 0:CAP], lhsT=WTall[:, tt, :], rhs=PgF[:, tt, :], start=(tt == 0), stop=(tt == 15)),
                           r=['WTall', 'Pg'], w=['pb0'], sig=(tt == 15))
                  s.op('dve', lambda e: e.tensor_copy(out=wtT[:], in_=pb[0][0:8, 0:CAP]), r=['pb0'], w=['wtT'])
                  for e_ in range(8):
                      s.op('pe', lambda e: e.matmul(pb[1][:, 0:CAP], lhsT=sel8[:, e_, :], rhs=wtT[:], start=True, stop=True),
                           r=['sel8', 'wtT'], w=['pb1'])
                      copy_on(evac_eng(), WTB[:, e_, :], pb[1][:, 0:CAP], ['pb1'], ['WTB'])
                  for e_ in range(8):
                      ex = g * 8 + e_
                      for fc in range(4):
                          for gu, wsrc in enumerate((w_gate, w_up)):
                              wt = wtl[wcount % 2]
                              wk = 'wtl%d' % (wcount % 2)
                              s.dma(('sp', 'act', 'pool')[wcount % 3], wt[:],
                                    wsrc[ex].rearrange("(j p) f -> p j f", p=128)[:, :, fc * 128:(fc + 1) * 128], w=[wk])
                              wcount += 1
                              pa = pb[2 + gu]
                              pk = 'pb%d' % (2 + gu)
                              for j in range(32):
                                  s.op('pe', lambda e: e.matmul(pa[:, 0:CAP], lhsT=wt[:, j, :], rhs=hTg[:, j, :], start=(j == 0), stop=(j == 31)),
                                       r=[wk, 'big'], w=[pk], sig=(j == 31))
                          s.op('act', lambda e: e.activation(out=sg[:], in_=pb[2][:, 0:CAP], func=AF.Silu), r=['pb2'], w=['sg'])
                          s.op('dve', lambda e: e.tensor_tensor(out=sg[:], in0=sg[:], in1=pb[3][:, 0:CAP], op=ALU.mult), r=['sg', 'pb3'], w=['sg'])
                          s.op('pool', lambda e: e.tensor_tensor(out=hbT[:, e_ * 4 + fc, :], in0=sg[:], in1=WTB[:, e_, :], op=ALU.mult),
                               r=['sg', 'WTB'], w=['hbT'])
                  wdv = w_down[g * 8:(g + 1) * 8].rearrange("e (fc p) d -> p (e fc) d", p=128)
                  for cc in range(16):
                      s.dma('sp' if cc % 2 else 'act', wd, wdv[:, :, cc * 256:(cc + 1) * 256], w=['big'])
                      for st_ in range(3):
                          pa = pb[4 + (ycount % 2)]
                          pk = 'pb%d' % (4 + (ycount % 2))
                          for k in range(32):
                              s.op('pe', lambda e: e.matmul(pa[:, 0:256], lhsT=hbT[:, k, st_ * 128:(st_ + 1) * 128], rhs=wd[:, k, :],
                                                            start=(k == 0), stop=(k == 31)), r=['hbT', 'big'], w=[pk], sig=(k == 31))
                          yb = yst[ycount % 2]
                          yk = 'yst%d' % (ycount % 2)
                          ycount += 1
                          copy_on(evac_eng(), yb[:], pa[:, 0:256], [pk], [yk])
                          row0 = g * CAP + st_ * 128
                          s.dma('pool', yav[row0:row0 + 128, cc * 256:(cc + 1) * 256], yb[:], r=[yk], w=['yall'])
              s.barrier()
              stop_after(6)

          for st in phase(7):
              def sb(name, shape, dt=F32):
                  return st.enter_context(nc.sbuf_tensor(name, shape, dt))

              def ps(name, shape, dt=F32):
                  return st.enter_context(nc.psum_tensor(name, shape, dt))
              R = sb("p7_R", [128, 32, 256])
              SQ = sb("p7_SQ", [128, 32, 256])
              PT = sb("p7_PT", [128, 24, 256], F32R)
              yl = [sb("p7_yl%d" % i, [128, 24, 128], F32R) for i in range(2)]
              otok = [sb("p7_otok%d" % i, [128, D]) for i in range(2)]
              posB = sb("p7_posB", [128, 256])
              GB = sb("p7_GB", [128, 8, 256])
              dg = sb("p7_dg", [128, 128])
              eqt = sb("p7_eq", [128, 256])
              stat = sb("p7_stat", [128, 256])
              pbc = ps("p7_pbc", [128, 128])
              pacc = [ps("p7_acc%d" % i, [128, 256]) for i in range(2)]
              pstat = ps("p7_pstat", [128, 256])
              ptr = [ps("p7_ptr%d" % i, [128, 128]) for i in range(2)]
              x1v = x1T.rearrange("(j p) t -> p j t", p=128)
              ylv = yall.rearrange("(s p) d -> p s d", p=128)
              tcount = 0
              for tq in range(8):
                  tsl = slice(tq * 256, (tq + 1) * 256)
                  s.dma('sp', R[:], x1v[:, :, tsl], r=['x1T'], w=['R'])
                  s.op('act', lambda e: e.activation(out=R[:], in_=R[:], func=AF.Identity, scale=ALPHA), r=['R'], w=['R'])
                  for tt in range(2):
                      tile = tq * 2 + tt
                      csl = slice(tt * 128, (tt + 1) * 128)
                      s.op('dve', lambda e: e.tensor_scalar(out=dg[:], in0=ident[:], scalar1=posall[:, tile:tile + 1], scalar2=None, op0=ALU.mult),
                           r=['ident', 'posall'], w=['dg'])
                      s.op('pe', lambda e: e.matmul(pbc[:], lhsT=onesF[:], rhs=dg[:], start=True, stop=True), r=['onesF', 'dg'], w=['pbc'])
                      s.op('act', lambda e: e.copy(out=posB[:, csl], in_=pbc[:]), r=['pbc'], w=['posB'])
                      for g in range(8):
                          s.op('dve', lambda e: e.tensor_scalar(out=dg[:], in0=ident[:], scalar1=Gall[:, tile, g:g + 1], scalar2=None, op0=ALU.mult),
                               r=['ident', 'Gall'], w=['dg'])
                          s.op('pe', lambda e: e.matmul(pbc[:], lhsT=onesF[:], rhs=dg[:], start=True, stop=True), r=['onesF', 'dg'], w=['pbc'])
                          s.op('act', lambda e: e.copy(out=GB[:, g, csl], in_=pbc[:]), r=['pbc'], w=['GB'])
                  for g in range(8):
                      for s3 in range(3):
                          s.op('dve', lambda e: e.tensor_scalar(out=eqt[:], in0=posB[:], scalar1=iotaP[:, s3:s3 + 1], scalar2=None, op0=ALU.is_equal),
                               r=['posB', 'iotaP'], w=['eqt'])
                          s.op('pool', lambda e: e.tensor_tensor(out=PT[:, g * 3 + s3, :], in0=eqt[:], in1=GB[:, g, :], op=ALU.mult),
                               r=['eqt', 'GB'], w=['PT'])
                  for j in range(32):
                      yb = yl[j % 2]
                      yk = 'yl%d' % (j % 2)
                      s.dma('sp' if j % 2 else 'act', yb[:], ylv[:, :, j * 128:(j + 1) * 128], r=['yall'], w=[yk])
                      pa = pacc[j % 2]
                      pk = 'acc%d' % (j % 2)
                      for s3 in range(24):
                          s.op('pe', lambda e: e.matmul(pa[:], lhsT=yb[:, s3, :], rhs=PT[:, s3, :], start=(s3 == 0), stop=(s3 == 23)),
                               r=[yk, 'PT'], w=[pk], sig=(s3 == 23))
                      s.op('dve', lambda e: e.scalar_tensor_tensor(out=R[:, j, :], in0=pa[:], scalar=g2p[:, j:j + 1], in1=R[:, j, :],
                                                                   op0=ALU.mult, op1=ALU.add), r=[pk, 'g2p', 'R'], w=['R'])
                  layer_norm(stat, R, SQ, 'R', 'SQ', pstat, ln2gT, ln2bT, 'ln2gT', 'ln2bT')
                  for tt in range(2):
                      ob = otok[tt]
                      ok = 'otok%d' % tt
                      t0 = tq * 256 + tt * 128
                      for j in range(32):
                          pt = ptr[tcount % 2]
                          pk = 'ptr%d' % (tcount % 2)
                          tcount += 1
                          s.op('pe', lambda e: e.transpose(pt[:], R[:, j, tt * 128:(tt + 1) * 128], ident[:]), r=['R', 'ident'], w=[pk])
                          copy_on(evac_eng(), ob[:, j * 128:(j + 1) * 128], pt[:], [pk], [ok])
                      s.dma('sp', out[t0:t0 + 128, :], ob[:], r=[ok], w=['out'])
              s.barrier()
    except _Stop:
        pass
    s.finish()
    nc._sch = s
    return nc


_NC = None


def make_shared(inputs):
    f = lambda a: np.ascontiguousarray(np.asarray(a, dtype=np.float32))
    return {
        'w_ada': f(inputs['w_ada'][0]),
        'b_ada': f(inputs['b_ada'][0]).reshape(192, 128),
        'w_in': f(inputs['w_in'][0]),
        'lam_re': f(inputs['ssm_lam_re'][0]), 'lam_im': f(inputs['ssm_lam_im'][0]),
        'log_dt': f(inputs['ssm_log_dt'][0]),
        'b_re': f(inputs['ssm_b_re'][0]), 'b_im': f(inputs['ssm_b_im'][0]),
        'c_re': f(inputs['ssm_c_re'][0]), 'c_im': f(inputs['ssm_c_im'][0]),
        'ssm_d': f(inputs['ssm_d'][0]).reshape(16, 128),
        'w_glu': f(inputs['w_glu'][0]), 'b_glu': f(inputs['b_glu'][0]).reshape(16, 128),
        'sink': f(inputs['attn_sink'][0]).reshape(1, 16),
        'w_out': f(inputs['w_out'][0]),
        'ln1_g': f(inputs['ln1_g'][0]).reshape(32, 128), 'ln1_b': f(inputs['ln1_b'][0]).reshape(32, 128),
        'w_r': f(np.concatenate([inputs['w_router_group'][0], inputs['w_router_expert'][0]], axis=1)),
        'b_r': f(np.concatenate([inputs['b_router_group'][0], inputs['b_router_expert'][0]], axis=0)).reshape(1, 72),
        'w_gate': f(inputs['w_gate_e'][0]), 'w_up': f(inputs['w_up_e'][0]), 'w_down': f(inputs['w_down_e'][0]),
        'ln2_g': f(inputs['ln2_g'][0]).reshape(32, 128), 'ln2_b': f(inputs['ln2_b'][0]).reshape(32, 128),
    }


def kernel(**inputs):
    global _NC
    f = lambda a: np.ascontiguousarray(np.asarray(a, dtype=np.float32))
    x = f(inputs['x']); c = f(inputs['c'])
    shared = make_shared(inputs)
    if _NC is None:
        _NC = build()
    in_maps = []
    for r in range(8):
        b = r % 4
        m = dict(shared)
        m['x'] = f(x[b])
        m['c'] = f(c[b]).reshape(32, 128)
        in_maps.append(m)
    res = run_bass_kernel_spmd(_NC, in_maps, core_ids=list(range(8)))
    outs = [np.asarray(res.results[b]['out'], dtype=np.float32).reshape(T, D) for b in range(4)]
    return np.stack(outs, axis=0)
```

```python
import contextlib
import math
import numpy as np
import concourse.bass as bass
import concourse.mybir as mybir
from concourse.bass_utils import run_bass_kernel_spmd

F32 = mybir.dt.float32
F32R = mybir.dt.float32r
I32 = mybir.dt.int32
ALU = mybir.AluOpType
AF = mybir.ActivationFunctionType
AX = mybir.AxisListType

T = 2048
D = 4096
NJ = 32
CAP = 384
ALPHA = 2.0 ** 0.25
EPS = 1e-5
TWO_PI = 2.0 * math.pi


class Sch:
    ENG = ['pe', 'dve', 'act', 'pool', 'sp']

    def __init__(self, nc, nds=12):
        self.nc = nc
        self.e = dict(pe=nc.tensor, dve=nc.vector, act=nc.scalar, pool=nc.gpsimd, sp=nc.sync)
        self.sem = {k: nc.alloc_semaphore(name='c_' + k) for k in self.ENG}
        self.cnt = {k: 0 for k in self.ENG}
        self.dsem = [nc.alloc_semaphore(name='d_%d' % i) for i in range(nds)]
        self.dcnt = [0] * nds
        self.dnext = 0
        self.seen = {k: {} for k in self.ENG}
        self.last_w = {}
        self.readers = {}

    def _wait(self, eng, tok):
        kind, a, v = tok
        if kind == 'c':
            if a == eng and (eng in ('pe', 'sp') or v > self.cnt[eng]):
                return
            key = ('c', a)
            sem = self.sem[a]
        else:
            key = ('d', a)
            sem = self.dsem[a]
        if self.seen[eng].get(key, 0) >= v:
            return
        self.seen[eng][key] = v
        self.e[eng].wait_ge(sem, v)

    def _deps(self, eng, r, w):
        toks = []
        for k in r:
            t = self.last_w.get(k)
            if t is not None:
                toks.append(t)
        for k in w:
            t = self.last_w.get(k)
            if t is not None:
                toks.append(t)
            toks.extend(self.readers.get(k, {}).values())
        for t in toks:
            self._wait(eng, t)

    def _record(self, tok, r, w):
        for k in r:
            d = self.readers.setdefault(k, {})
            d[(tok[0], tok[1])] = tok
        for k in w:
            self.last_w[k] = tok
            self.readers[k] = {}

    def op(self, eng, fn, r=(), w=(), sig=True):
        self._deps(eng, r, w)
        ins = fn(self.e[eng])
        if sig:
            self.cnt[eng] += 1
            ins.then_inc(self.sem[eng], 1)
            tok = ('c', eng, self.cnt[eng])
        else:
            tok = ('c', eng, self.cnt[eng] + 1)
        self._record(tok, r, w)
        return ins

    def dma(self, eng, out, in_, r=(), w=(), **kw):
        if eng == 'pool':
            eng = 'sp'
        self._deps(eng, r, w)
        i = self.dnext
        self.dnext = (self.dnext + 1) % len(self.dsem)
        if self.dcnt[i] > 0:
            self._wait(eng, ('d', i, self.dcnt[i]))
        ins = self.e[eng].dma_start(out=out, in_=in_, **kw)
        self.dcnt[i] += 16
        ins.then_inc(self.dsem[i], 16)
        tok = ('d', i, self.dcnt[i])
        self._record(tok, r, w)
        return ins

    def barrier(self):
        for eng in self.ENG:
            for a in self.ENG:
                if a != eng and self.cnt[a] > 0:
                    self._wait(eng, ('c', a, self.cnt[a]))
            for i, v in enumerate(self.dcnt):
                if v > 0:
                    self._wait(eng, ('d', i, v))
        self.last_w = {}
        self.readers = {}

    def finish(self):
        for i, v in enumerate(self.dcnt):
            if v > 0:
                self._wait('sp', ('d', i, v))
        for a in self.ENG:
            if a != 'sp' and self.cnt[a] > 0:
                self._wait('sp', ('c', a, self.cnt[a]))


class _Stop(Exception):
    pass


def build(upto=7, debug=False, start=1, sub=0):
    nc = bass.Bass("TRN2", target_bir_lowering=False)
    nc.dge_precook = False
    s = Sch(nc)

    def din(name, shape, dt=F32, used=(1, 2, 3, 4, 5, 6, 7)):
        if not any(start <= p <= upto for p in used):
            return None
        return nc.dram_tensor(name, shape, dt, kind="ExternalInput").ap()

    def dscr(name, shape, dt=F32, made=1, used=(7,)):
        if (max(made) if isinstance(made, tuple) else made) < start:
            if not any(start <= p <= upto for p in used):
                return None
            return nc.dram_tensor(name, shape, dt, kind="ExternalInput").ap()
        return nc.dram_tensor(name, shape, dt, kind="ExternalOutput" if debug else "Internal").ap()

    def ddbg(name, shape, dt=F32):
        return nc.dram_tensor(name, shape, dt, kind="ExternalOutput").ap() if debug else None

    x = din("x", [T, D], used=(2, 5))
    cvec = din("c", [32, 128], used=(1,))
    w_ada = din("w_ada", [D, 6 * D], F32R, used=(1,))
    b_ada = din("b_ada", [192, 128])
    w_in = din("w_in", [D, 5120], F32R, used=(2,))
    lam_re = din("lam_re", [2, 128, 64], used=(3,))
    lam_im = din("lam_im", [2, 128, 64], used=(3,))
    log_dt = din("log_dt", [2, 128], used=(3,))
    b_re = din("b_re", [2, 128, 64, 16], used=(3,))
    b_im = din("b_im", [2, 128, 64, 16], used=(3,))
    c_re = din("c_re", [2, 128, 16, 64], used=(3,))
    c_im = din("c_im", [2, 128, 16, 64], used=(3,))
    ssm_d = din("ssm_d", [16, 128])
    w_glu = din("w_glu", [2048, 2048], F32R, used=(5,))
    b_glu = din("b_glu", [16, 128])
    sink = din("sink", [1, 16], used=(4,))
    w_out = din("w_out", [D, D], F32R, used=(5,))
    ln1_g = din("ln1_g", [32, 128])
    ln1_b = din("ln1_b", [32, 128])
    w_r = din("w_r", [D, 72], used=(5,))
    b_r = din("b_r", [1, 72], used=(5,))
    w_gate = din("w_gate", [64, D, 512], F32R, used=(6,))
    w_up = din("w_up", [64, D, 512], F32R, used=(6,))
    w_down = din("w_down", [64, 512, D], F32R, used=(6,))
    ln2_g = din("ln2_g", [32, 128])
    ln2_b = din("ln2_b", [32, 128])
    out = nc.dram_tensor("out", [T, D], F32, kind="ExternalOutput").ap()

    modrow = dscr("modrow", [192, 128], made=1, used=(1, 2, 3, 4, 5, 6, 7))
    projT = dscr("projT", [5120, T], F32R, made=2, used=(3, 4))
    ymixT = dscr("ymixT", [4096, T], made=(3, 4), used=(5,))
    x1T = dscr("x1T", [D, T], made=5, used=(7,))
    h2tm = dscr("h2tm", [T, D], F32R, made=5, used=(6,))
    yall = dscr("yall", [8 * CAP, D], F32R, made=6, used=(7,))

    dbg_modT = ddbg("dbg_modT", [128, 192])
    dbg_lb = ddbg("dbg_lb", [128, 256])
    dbg_G = ddbg("dbg_G", [128, 128])
    dbg_W = ddbg("dbg_W", [128, 128])
    dbg_pos = ddbg("dbg_pos", [128, 16])

    def phase(n):
        if start <= n <= upto:
            with contextlib.ExitStack() as st_:
                yield st_

    def sub_stop(k):
        if sub == k:
            s.barrier()
            raise _Stop()

    def stop_after(n):
        if upto == n:
            raise _Stop()

    rr = [0]

    def evac_eng():
        rr[0] ^= 1
        return 'act' if rr[0] else 'dve'

    def copy_on(eng, out_ap, in_ap, r, w):
        if eng == 'act':
            s.op('act', lambda e: e.copy(out=out_ap, in_=in_ap), r=r, w=w)
        else:
            s.op(eng, lambda e: e.tensor_copy(out=out_ap, in_=in_ap), r=r, w=w)

    try:
      with contextlib.ExitStack() as gst:
          def gsb(name, shape, dt=F32):
              return gst.enter_context(nc.sbuf_tensor(name, shape, dt))

          ident = gsb("ident", [128, 128])
          onesF = gsb("onesF", [128, 128])
          onesM = gsb("onesM", [128, 128])
          onesR = gsb("onesR", [128, 128], F32R)
          ustrict = gsb("ustrict", [128, 128])
          blockmask = gsb("blockmask", [128, 128])
          rowmask = gsb("rowmask", [128, 8])
          colmask = gsb("colmask", [128, 8, 128])
          sel8 = gsb("sel8", [8, 8, 128])
          iotaS = gsb("iotaS", [128, CAP])
          iotaP = gsb("iotaP", [128, 3])
          modT = gsb("modT", [128, 192])
          badT = gsb("badT", [128, 192])
          sc1p = gsb("sc1p", [128, 32]); g1p = gsb("g1p", [128, 32])
          sc2p = gsb("sc2p", [128, 32]); g2p = gsb("g2p", [128, 32])
          ln1gT = gsb("ln1gT", [128, 32]); ln1bT = gsb("ln1bT", [128, 32])
          ln2gT = gsb("ln2gT", [128, 32]); ln2bT = gsb("ln2bT", [128, 32])
          bgluT = gsb("bgluT", [128, 16]); dT = gsb("dT", [128, 16])
          Gall = gsb("Gall", [128, 16, 8]); WTall = gsb("WTall", [128, 16, 8])
          posall = gsb("posall", [128, 16])

          s.op('pool', lambda e: e.memset(ident[:], 0.0), w=['ident'])
          s.op('pool', lambda e: e.affine_select(out=ident[:], in_=ident[:], pattern=[[-1, 128]],
                                                 compare_op=ALU.not_equal, fill=1.0, base=0, channel_multiplier=1),
               r=['ident'], w=['ident'])
          s.op('pool', lambda e: e.memset(onesF[:], 1.0), w=['onesF'])
          s.op('pool', lambda e: e.memset(onesM[:], 1.0 / D), w=['onesM'])
          s.op('dve', lambda e: e.tensor_copy(out=onesR[:], in_=onesF[:]), r=['onesF'], w=['onesR'])
          s.op('pool', lambda e: e.memset(ustrict[:], 1.0), w=['ustrict'])
          s.op('pool', lambda e: e.affine_select(out=ustrict[:], in_=ustrict[:], pattern=[[1, 128]],
                                                 compare_op=ALU.is_gt, fill=0.0, base=0, channel_multiplier=-1),
               r=['ustrict'], w=['ustrict'])
          s.op('pool', lambda e: e.memset(rowmask[:], 1.0), w=['rowmask'])
          s.op('pool', lambda e: e.affine_select(out=rowmask[:], in_=rowmask[:], pattern=[[-16, 8]],
                                                 compare_op=ALU.is_ge, fill=0.0, base=0, channel_multiplier=1),
               r=['rowmask'], w=['rowmask'])
          s.op('pool', lambda e: e.affine_select(out=rowmask[:], in_=rowmask[:], pattern=[[16, 8]],
                                                 compare_op=ALU.is_ge, fill=0.0, base=15, channel_multiplier=-1),
               r=['rowmask'], w=['rowmask'])
          s.op('pool', lambda e: e.memset(colmask[:], 1.0), w=['colmask'])
          s.op('pool', lambda e: e.affine_select(out=colmask[:], in_=colmask[:], pattern=[[-16, 8], [1, 128]],
                                                 compare_op=ALU.is_ge, fill=0.0, base=0, channel_multiplier=0),
               r=['colmask'], w=['colmask'])
          s.op('pool', lambda e: e.affine_select(out=colmask[:], in_=colmask[:], pattern=[[16, 8], [-1, 128]],
                                                 compare_op=ALU.is_ge, fill=0.0, base=15, channel_multiplier=0),
               r=['colmask'], w=['colmask'])
          with nc.sbuf_tensor("bm_tmp", [128, 8, 128], F32) as bmt:
              s.op('pool', lambda e: e.tensor_tensor(out=bmt[:], in0=colmask[:],
                                                     in1=rowmask[:].unsqueeze(2).to_broadcast([128, 8, 128]), op=ALU.mult),
                   r=['colmask', 'rowmask'], w=['bmt'])
              s.op('dve', lambda e: e.tensor_reduce(out=blockmask[:], in_=bmt[:].rearrange("p g q -> p q g"),
                                                    axis=AX.X, op=ALU.add), r=['bmt'], w=['blockmask'])
              s.barrier()
          s.op('pool', lambda e: e.memset(sel8[:], 1.0), w=['sel8'])
          s.op('pool', lambda e: e.affine_select(out=sel8[:], in_=sel8[:], pattern=[[-1, 8], [0, 128]],
                                                 compare_op=ALU.is_equal, fill=0.0, base=0, channel_multiplier=1),
               r=['sel8'], w=['sel8'])
          s.op('pool', lambda e: e.iota(iotaS[:], pattern=[[1, CAP]], base=0, channel_multiplier=0,
                                        allow_small_or_imprecise_dtypes=True), w=['iotaS'])
          s.op('pool', lambda e: e.iota(iotaP[:], pattern=[[128, 3]], base=0, channel_multiplier=1,
                                        allow_small_or_imprecise_dtypes=True), w=['iotaP'])

          with contextlib.ExitStack() as st:
              def sb(name, shape, dt=F32):
                  return st.enter_context(nc.sbuf_tensor(name, shape, dt))

              def ps(name, shape, dt=F32):
                  return st.enter_context(nc.psum_tensor(name, shape, dt))
              stg = sb("p1_stg", [128, 128])
              pT = ps("p1_pT", [128, 128])
              cT = sb("p1_cT", [128, 32], F32R)
              pm = ps("p1_pm", [1, 256])
              mrow = sb("p1_mrow", [1, 256])
              wa = [sb("p1_wa%d" % i, [128, 32, 256], F32R) for i in range(2)]

              def load_vecT(dst, src, rows, func=None, dstkey=None):
                  s.dma('sp', stg[0:rows, :], src, w=['stg'])
                  s.op('pe', lambda e: e.transpose(pT[:, 0:rows], stg[0:rows, :], ident[0:rows, 0:rows]),
                       r=['stg', 'ident'], w=['pT'])
                  if func is None:
                      s.op('dve', lambda e: e.tensor_copy(out=dst, in_=pT[:, 0:rows]), r=['pT'], w=[dstkey])
                  else:
                      s.op('act', lambda e: e.activation(out=dst, in_=pT[:, 0:rows], func=func), r=['pT'], w=[dstkey])

              if start <= 1:
                  load_vecT(cT[:], cvec[:, :], 32, func=AF.Silu, dstkey='cT')
              load_vecT(badT[:, 0:96], b_ada[0:96, :], 96, dstkey='badT')
              load_vecT(badT[:, 96:192], b_ada[96:192, :], 96, dstkey='badT')
              load_vecT(ln1gT[:], ln1_g[:, :], 32, dstkey='ln1gT')
              load_vecT(ln1bT[:], ln1_b[:, :], 32, dstkey='ln1bT')
              load_vecT(ln2gT[:], ln2_g[:, :], 32, dstkey='ln2gT')
              load_vecT(ln2bT[:], ln2_b[:, :], 32, dstkey='ln2bT')
              load_vecT(bgluT[:], b_glu[:, :], 16, dstkey='bgluT')
              load_vecT(dT[:], ssm_d[:, :], 16, dstkey='dT')

              if start <= 1:
                  wav = w_ada.rearrange("(j p) m -> p j m", p=128)
                  mrflat = modrow.rearrange("a b -> (a b)")
                  for n in range(96):
                      wt = wa[n % 2]
                      k = 'wa%d' % (n % 2)
                      s.dma('sp' if n % 2 == 0 else 'act', wt[:], wav[:, :, n * 256:(n + 1) * 256], w=[k])
                      for j in range(32):
                          s.op('pe', lambda e: e.matmul(pm[:], lhsT=cT[:, j:j + 1], rhs=wt[:, j, :],
                                                        start=(j == 0), stop=(j == 31)),
                               r=[k, 'cT'], w=['pm'], sig=(j == 31))
                      s.op('dve', lambda e: e.tensor_copy(out=mrow[:], in_=pm[:]), r=['pm'], w=['mrow'])
                      s.dma('sp', mrflat[n * 256:(n + 1) * 256].unsqueeze(0), mrow[:], r=['mrow'], w=['modrow'])
              load_vecT(modT[:, 0:96], modrow[0:96, :], 96, dstkey='modT')
              load_vecT(modT[:, 96:192], modrow[96:192, :], 96, dstkey='modT')
              s.op('dve', lambda e: e.tensor_tensor(out=modT[:], in0=modT[:], in1=badT[:], op=ALU.add),
                   r=['modT', 'badT'], w=['modT'])
              for dst, q, key in ((sc1p, 1, 'sc1p'), (g1p, 2, 'g1p'), (sc2p, 4, 'sc2p'), (g2p, 5, 'g2p')):
                  s.op('dve', lambda e: e.tensor_scalar(out=dst[:], in0=modT[:, q * 32:(q + 1) * 32], scalar1=1.0,
                                                        scalar2=None, op0=ALU.add), r=['modT'], w=[key])
              if debug:
                  s.dma('sp', dbg_modT, modT[:], r=['modT'], w=['dbg_modT'])
              s.barrier()
              stop_after(1)
          sh1 = modT[:, 0:32]
          sh2 = modT[:, 96:128]

          for st in phase(2):
              def sb(name, shape, dt=F32):
                  return st.enter_context(nc.sbuf_tensor(name, shape, dt))

              def ps(name, shape, dt=F32):
                  return st.enter_context(nc.psum_tensor(name, shape, dt))
              hT = sb("p2_hT", [128, 32, 512], F32R)
              xs = [sb("p2_xs%d" % i, [128, D]) for i in range(2)]
              wt2 = [sb("p2_w%d" % i, [128, 32, 256], F32R) for i in range(2)]
              ost = [sb("p2_o%d" % i, [128, 512], F32R) for i in range(2)]
              ptr = [ps("p2_ptr%d" % i, [128, 128]) for i in range(3)]
              pacc = [ps("p2_acc%d" % i, [128, 512]) for i in range(2)]
              wv = w_in.rearrange("(j p) m -> p j m", p=128)
              tcount = 0
              ocount = 0
              for tc in range(4):
                  for tt in range(4):
                      xb = xs[tt % 2]
                      xk = 'xs%d' % (tt % 2)
                      t0 = tc * 512 + tt * 128
                      s.dma('sp', xb[:], x[t0:t0 + 128, :], w=[xk])
                      for j in range(32):
                          pt = ptr[tcount % 3]
                          pk = 'ptr%d' % (tcount % 3)
                          tcount += 1
                          s.op('pe', lambda e: e.transpose(pt[:], xb[:, j * 128:(j + 1) * 128], ident[:]),
                               r=[xk, 'ident'], w=[pk])
                          dst = hT[:, j, tt * 128:(tt + 1) * 128]
                          if j % 2 == 0:
                              s.op('act', lambda e: e.activation(out=dst, in_=pt[:], func=AF.Identity,
                                                                 bias=sh1[:, j:j + 1], scale=sc1p[:, j:j + 1]),
                                   r=[pk, 'modT', 'sc1p'], w=['hT'])
                          else:
                              s.op('dve', lambda e: e.tensor_scalar(out=dst, in0=pt[:], scalar1=sc1p[:, j:j + 1],
                                                                    scalar2=sh1[:, j:j + 1], op0=ALU.mult, op1=ALU.add),
                                   r=[pk, 'modT', 'sc1p'], w=['hT'])
                  for mp in range(20):
                      wt = wt2[mp % 2]
                      wk = 'w2_%d' % (mp % 2)
                      s.dma('sp' if mp % 2 == 0 else 'act', wt[:], wv[:, :, mp * 256:(mp + 1) * 256], w=[wk])
                      for mh in range(2):
                          pa = pacc[mh]
                          pk = 'acc%d' % mh
                          for j in range(32):
                              s.op('pe', lambda e: e.matmul(pa[:], lhsT=wt[:, j, mh * 128:(mh + 1) * 128], rhs=hT[:, j, :],
                                                            start=(j == 0), stop=(j == 31)),
                                   r=[wk, 'hT'], w=[pk], sig=(j == 31))
                          ob = ost[ocount % 2]
                          ok = 'ost%d' % (ocount % 2)
                          ocount += 1
                          copy_on(evac_eng(), ob[:], pa[:], [pk], [ok])
                          m = mp * 2 + mh
                          s.dma('pool', projT[m * 128:(m + 1) * 128, tc * 512:(tc + 1) * 512], ob[:], r=[ok], w=['projT'])
              s.barrier()
              stop_after(2)

          for st in phase(3):
              def sb(name, shape, dt=F32):
                  return st.enter_context(nc.sbuf_tensor(name, shape, dt))

              def ps(name, shape, dt=F32):
                  return st.enter_context(nc.psum_tensor(name, shape, dt))
              pT3 = ps("p3_pT", [128, 128])
              pkb = ps("p3_pkb", [128, 128])
              pst = [ps("p3_pst%d" % i, [128, 256]) for i in range(2)]
              py3 = [ps("p3_py%d" % i, [128, 256]) for i in range(2)]
              stg3 = sb("p3_stg", [128, 2, 64])
              lr = sb("p3_lr", [128, 128]); li = sb("p3_li", [128, 128]); dtb = sb("p3_dt", [128, 128])
              t1 = sb("p3_t1", [128, 128]); t2 = sb("p3_t2", [128, 128]); t3 = sb("p3_t3", [128, 128])
              ki = sb("p3_ki", [128, 128], I32)
              lbr = sb("p3_lbr", [128, 128]); lbi = sb("p3_lbi", [128, 128])
              cr = sb("p3_cr", [128, 128]); ci = sb("p3_ci", [128, 128])
              Akr = sb("p3_Akr", [128, 9, 128]); Aki = sb("p3_Aki", [128, 9, 128])
              PWr = sb("p3_PWr", [128, 8, 128]); PWi = sb("p3_PWi", [128, 8, 128])
              PHr = sb("p3_PHr", [128, 8, 128]); PHi = sb("p3_PHi", [128, 8, 128])

              def vop(eng, out_ap, a, b, op, r, w):
                  s.op(eng, lambda e: e.tensor_tensor(out=out_ap, in0=a, in1=b, op=op), r=r, w=w)

              def load_pg(dst, src, key):
                  s.dma('sp', stg3[:], src.rearrange("d g p -> g d p"), w=['stg3'])
                  s.op('pe', lambda e: e.transpose(pT3[:], stg3[:].rearrange("g d p -> g (d p)"), ident[:]),
                       r=['stg3', 'ident'], w=['pT3'])
                  s.op('dve', lambda e: e.tensor_copy(out=dst[:], in_=pT3[:]), r=['pT3'], w=[key])
              load_pg(lr, lam_re, 'lr')
              load_pg(li, lam_im, 'li')
              for d in range(2):
                  s.dma('sp', dtb[d * 64:(d + 1) * 64, :], log_dt[d, :].partition_broadcast(64), w=['dtb'])
              s.op('act', lambda e: e.activation(out=dtb[:], in_=dtb[:], func=AF.Exp), r=['dtb'], w=['dtb'])
              sub_stop(1)
              vop('dve', t1[:], lr[:], dtb[:], ALU.mult, ['lr', 'dtb'], ['t1'])
              s.op('act', lambda e: e.activation(out=t1[:], in_=t1[:], func=AF.Exp), r=['t1'], w=['t1'])
              vop('dve', t2[:], li[:], dtb[:], ALU.mult, ['li', 'dtb'], ['t2'])

              def sin_of(dst, shift, key):
                  s.op('dve', lambda e: e.tensor_scalar(out=t3[:], in0=t2[:], scalar1=1.0 / TWO_PI,
                                                        scalar2=shift / TWO_PI + 0.5, op0=ALU.mult, op1=ALU.add),
                       r=['t2'], w=['t3'])
                  s.op('dve', lambda e: e.tensor_copy(out=ki[:], in_=t3[:]), r=['t3'], w=['ki'])
                  s.op('dve', lambda e: e.tensor_copy(out=dst[:], in_=ki[:]), r=['ki'], w=[key])
                  s.op('dve', lambda e: e.tensor_tensor(out=t3[:], in0=dst[:], in1=t3[:], op=ALU.is_gt),
                       r=[key, 't3'], w=['t3'])
                  vop('dve', dst[:], dst[:], t3[:], ALU.subtract, [key, 't3'], [key])
                  s.op('dve', lambda e: e.scalar_tensor_tensor(out=dst[:], in0=dst[:], scalar=-TWO_PI, in1=t2[:],
                                                               op0=ALU.mult, op1=ALU.add), r=[key, 't2'], w=[key])
                  s.op('dve', lambda e: e.tensor_scalar(out=dst[:], in0=dst[:], scalar1=float(shift), scalar2=math.pi,
                                                        op0=ALU.add, op1=ALU.min), r=[key], w=[key])
                  s.op('dve', lambda e: e.tensor_scalar(out=dst[:], in0=dst[:], scalar1=-math.pi, scalar2=None,
                                                        op0=ALU.max), r=[key], w=[key])
                  s.op('act', lambda e: e.activation(out=dst[:], in_=dst[:], func=AF.Sin), r=[key], w=[key])
              sin_of(lbi, 0.0, 'lbi')
              sub_stop(2)
              sin_of(lbr, math.pi / 2, 'lbr')
              vop('dve', lbr[:], lbr[:], t1[:], ALU.mult, ['lbr', 't1'], ['lbr'])
              vop('dve', lbi[:], lbi[:], t1[:], ALU.mult, ['lbi', 't1'], ['lbi'])
              if debug:
                  s.dma('sp', dbg_lb[:, 0:128], lbr[:], r=['lbr'], w=['dbg_lb'])
                  s.dma('sp', dbg_lb[:, 128:256], lbi[:], r=['lbi'], w=['dbg_lb'])
              vop('dve', t1[:], lr[:], lr[:], ALU.mult, ['lr'], ['t1'])
              vop('dve', t2[:], li[:], li[:], ALU.mult, ['li'], ['t2'])
              vop('dve', t1[:], t1[:], t2[:], ALU.add, ['t1', 't2'], ['t1'])
              s.op('dve', lambda e: e.reciprocal(out=t1[:], in_=t1[:]), r=['t1'], w=['t1'])
              s.op('dve', lambda e: e.tensor_scalar(out=t2[:], in0=lbr[:], scalar1=-1.0, scalar2=None, op0=ALU.add),
                   r=['lbr'], w=['t2'])
              vop('dve', cr[:], t2[:], lr[:], ALU.mult, ['t2', 'lr'], ['cr'])
              vop('dve', t3[:], lbi[:], li[:], ALU.mult, ['lbi', 'li'], ['t3'])
              vop('dve', cr[:], cr[:], t3[:], ALU.add, ['cr', 't3'], ['cr'])
              vop('dve', cr[:], cr[:], t1[:], ALU.mult, ['cr', 't1'], ['cr'])
              vop('dve', ci[:], lbi[:], lr[:], ALU.mult, ['lbi', 'lr'], ['ci'])
              vop('dve', t3[:], t2[:], li[:], ALU.mult, ['t2', 'li'], ['t3'])
              vop('dve', ci[:], ci[:], t3[:], ALU.subtract, ['ci', 't3'], ['ci'])
              vop('dve', ci[:], ci[:], t1[:], ALU.mult, ['ci', 't1'], ['ci'])
              sub_stop(3)
              s.op('dve', lambda e: e.memset(Akr[:, 0, :], 1.0), w=['Akr'])
              s.op('dve', lambda e: e.memset(Aki[:, 0, :], 0.0), w=['Aki'])
              for k in range(1, 9):
                  vop('dve', t1[:], Akr[:, k - 1, :], lbr[:], ALU.mult, ['Akr', 'lbr'], ['t1'])
                  vop('dve', t2[:], Aki[:, k - 1, :], lbi[:], ALU.mult, ['Aki', 'lbi'], ['t2'])
                  vop('dve', Akr[:, k, :], t1[:], t2[:], ALU.subtract, ['t1', 't2'], ['Akr'])
                  vop('dve', t1[:], Akr[:, k - 1, :], lbi[:], ALU.mult, ['Akr', 'lbi'], ['t1'])
                  vop('dve', t2[:], Aki[:, k - 1, :], lbr[:], ALU.mult, ['Aki', 'lbr'], ['t2'])
                  vop('dve', Aki[:, k, :], t1[:], t2[:], ALU.add, ['t1', 't2'], ['Aki'])
              sub_stop(32)
              for j in range(8):
                  for (dstp, srcp, kd, ks) in ((PWr, Akr, 'PWr', 'Akr'), (PWi, Aki, 'PWi', 'Aki'),
                                               (PHr, Akr, 'PHr', 'Akr'), (PHi, Aki, 'PHi', 'Aki')):
                      if kd.startswith('PW'):
                          ef, eb = 7 - j, j
                      else:
                          ef, eb = j + 1, 8 - j
                      s.op('dve', lambda e: e.tensor_copy(out=dstp[0:64, j, :], in_=srcp[0:64, ef, :]), r=[ks], w=[kd])
                      s.op('dve', lambda e: e.tensor_copy(out=dstp[64:128, j, :], in_=srcp[64:128, eb, :]), r=[ks], w=[kd])

              Ppr = sb("p3_Ppr", [128, 16, 128]); Ppi = sb("p3_Ppi", [128, 16, 128])
              c8r = sb("p3_c8r", [128, 128]); c8i = sb("p3_c8i", [128, 128])
              s.op('dve', lambda e: e.tensor_copy(out=c8r[:], in_=Akr[:, 8, :]), r=['Akr'], w=['c8r'])
              s.op('dve', lambda e: e.tensor_copy(out=c8i[:], in_=Aki[:, 8, :]), r=['Aki'], w=['c8i'])
              for m_ in range(1, 17):
                  for (dstp, srcp, kd, ks) in ((Ppr, c8r, 'Ppr', 'c8r'), (Ppi, c8i, 'Ppi', 'c8i')):
                      s.op('dve', lambda e: e.tensor_copy(out=dstp[0:64, m_ - 1, :], in_=srcp[0:64, :]), r=[ks], w=[kd])
                      s.op('pool', lambda e: e.tensor_copy(out=dstp[64:128, 16 - m_, :], in_=srcp[64:128, :]), r=[ks], w=[kd])
                  if m_ < 16:
                      vop('dve', t1[:], c8r[:], Akr[:, 8, :], ALU.mult, ['c8r', 'Akr'], ['t1'])
                      vop('dve', t2[:], c8i[:], Aki[:, 8, :], ALU.mult, ['c8i', 'Aki'], ['t2'])
                      vop('dve', t3[:], c8r[:], Aki[:, 8, :], ALU.mult, ['c8r', 'Aki'], ['t3'])
                      vop('dve', c8i[:], c8i[:], Akr[:, 8, :], ALU.mult, ['c8i', 'Akr'], ['c8i'])
                      vop('dve', c8r[:], t1[:], t2[:], ALU.subtract, ['t1', 't2'], ['c8r'])
                      vop('dve', c8i[:], c8i[:], t3[:], ALU.add, ['c8i', 't3'], ['c8i'])
              sub_stop(4)
              mkf = sb("p3_mkf", [128, 2])
              s.op('pool', lambda e: e.memset(mkf[:, :], 0.0), w=['mkf'])
              s.op('pool', lambda e: e.memset(mkf[0:64, 0:1], 1.0), w=['mkf'])
              s.op('pool', lambda e: e.memset(mkf[64:128, 1:2], 1.0), w=['mkf'])
              Cmk = [sb("p3_Cmk%d" % i, [128, 128]) for i in range(4)]
              Zr = sb("p3_Zr", [128, 8, 256]); Zi = sb("p3_Zi", [128, 8, 256])
              ZRr = sb("p3_ZRr", [128, 8, 256], F32R); ZRi = sb("p3_ZRi", [128, 8, 256], F32R)
              tm = [sb("p3_tm%d" % i, [128, 8, 16]) for i in range(4)]
              Cnr = sb("p3_Cnr", [128, 8, 16]); Cni = sb("p3_Cni", [128, 8, 16])
              Hs = [[sb("p3_H%d_%d" % (c_, x_), [128, 8, 128]) for x_ in range(2)] for c_ in range(2)]
              KBs = [sb("p3_KB%d" % c_, [128, 15, 128], F32R) for c_ in range(2)]
              Bre = sb("p3_Bre", [128, 8, 16]); Bim = sb("p3_Bim", [128, 8, 16])
              Bbr = sb("p3_Bbr", [128, 8, 16]); Bbi = sb("p3_Bbi", [128, 8, 16])
              tb1 = sb("p3_tb1", [128, 8, 16]); tb2 = sb("p3_tb2", [128, 8, 16])
              Cst = sb("p3_Cst", [128, 2, 64])
              Cre = sb("p3_Cre", [128, 128]); Cim = sb("p3_Cim", [128, 128]); nCim = sb("p3_nCim", [128, 128])
              ABr = sb("p3_ABr", [128, 8, 128]); ABi = sb("p3_ABi", [128, 8, 128])
              tab1 = sb("p3_tab1", [128, 8, 128]); tab2 = sb("p3_tab2", [128, 8, 128])
              ABT = sb("p3_ABT", [128, 8, 2, 128])
              GTg = sb("p3_GTg", [128, 8, 2, 128], F32R)
              HM = GTg
              uT = [sb("p3_uT%d" % c_, [128, T], F32R) for c_ in range(2)]
              yT = sb("p3_yT", [128, T])

              for gb in range(16):
                  G0 = gb * 8
                  s.op('dve', lambda e: e.memset(Zr[:, :, :], 0.0), w=['Zrf', 'Zrb'])
                  s.op('pool', lambda e: e.memset(Zi[:, :, :], 0.0), w=['Zif', 'Zib'])
                  for cl in (gb % 2,):
                      ct = gb
                      g0 = ct * 8
                      uk = 'uT%d' % cl
                      s.dma('sp', uT[cl][:], projT[ct * 128:(ct + 1) * 128, :], r=['projT'], w=[uk])
                      for d in range(2):
                          s.dma('act', Bre[d * 64:(d + 1) * 64, :, :], b_re[d, g0:g0 + 8, :, :].rearrange("g p h -> p g h"), w=['Bre'])
                          s.dma('act', Bim[d * 64:(d + 1) * 64, :, :], b_im[d, g0:g0 + 8, :, :].rearrange("g p h -> p g h"), w=['Bim'])
                      for (src, dst, key) in ((c_re, Cre, 'Cre'), (c_im, Cim, 'Cim')):
                          s.dma('sp', Cst[:], src[:, g0:g0 + 8, :, :].rearrange("d g h p -> (g h) d p"), w=['Cst'])
                          s.op('pe', lambda e: e.transpose(pT3[:], Cst[:].rearrange("q d p -> q (d p)"), ident[:]),
                               r=['Cst', 'ident'], w=['pT3'])
                          s.op('dve', lambda e: e.tensor_copy(out=dst[:], in_=pT3[:]), r=['pT3'], w=[key])
                      s.op('dve', lambda e: e.tensor_scalar(out=nCim[:], in0=Cim[:], scalar1=-1.0, scalar2=None, op0=ALU.mult),
                           r=['Cim'], w=['nCim'])
                      crb = cr[:, g0:g0 + 8].unsqueeze(2).to_broadcast([128, 8, 16])
                      cib = ci[:, g0:g0 + 8].unsqueeze(2).to_broadcast([128, 8, 16])
                      vop('dve', tb1[:], Bre[:], crb, ALU.mult, ['Bre', 'cr'], ['tb1'])
                      vop('dve', tb2[:], Bim[:], cib, ALU.mult, ['Bim', 'ci'], ['tb2'])
                      vop('dve', Bbr[:], tb1[:], tb2[:], ALU.subtract, ['tb1', 'tb2'], ['Bbr'])
                      vop('dve', tb1[:], Bim[:], crb, ALU.mult, ['Bim', 'cr'], ['tb1'])
                      vop('dve', tb2[:], Bre[:], cib, ALU.mult, ['Bre', 'ci'], ['tb2'])
                      vop('dve', Bbi[:], tb1[:], tb2[:], ALU.add, ['tb1', 'tb2'], ['Bbi'])
                      pwr = PWr[:, :, g0:g0 + 8].unsqueeze(3).to_broadcast([128, 8, 8, 16])
                      pwi = PWi[:, :, g0:g0 + 8].unsqueeze(3).to_broadcast([128, 8, 8, 16])
                      bbr = Bbr[:].unsqueeze(1).to_broadcast([128, 8, 8, 16])
                      bbi = Bbi[:].unsqueeze(1).to_broadcast([128, 8, 8, 16])
                      v4 = lambda tl: tl[:].rearrange("p j (g h) -> p j g h", h=16)
                      vop('dve', v4(tab1), pwr, bbr, ALU.mult, ['PWr', 'Bbr'], ['tab1'])
                      vop('pool', v4(tab2), pwi, bbi, ALU.mult, ['PWi', 'Bbi'], ['tab2'])
                      vop('dve', ABr[:], tab1[:], tab2[:], ALU.subtract, ['tab1', 'tab2'], ['ABr'])
                      vop('dve', v4(tab1), pwr, bbi, ALU.mult, ['PWr', 'Bbi'], ['tab1'])
                      vop('pool', v4(tab2), pwi, bbr, ALU.mult, ['PWi', 'Bbr'], ['tab2'])
                      vop('dve', ABi[:], tab1[:], tab2[:], ALU.add, ['tab1', 'tab2'], ['ABi'])
                      phr = PHr[:, :, g0:g0 + 8].unsqueeze(3).to_broadcast([128, 8, 8, 16])
                      phi = PHi[:, :, g0:g0 + 8].unsqueeze(3).to_broadcast([128, 8, 8, 16])
                      creb = Cre[:].rearrange("p (g h) -> p g h", h=16).unsqueeze(1).to_broadcast([128, 8, 8, 16])
                      cimb = Cim[:].rearrange("p (g h) -> p g h", h=16).unsqueeze(1).to_broadcast([128, 8, 8, 16])
                      ncimb = nCim[:].rearrange("p (g h) -> p g h", h=16).unsqueeze(1).to_broadcast([128, 8, 8, 16])
                      HR, HI = Hs[cl]
                      hk = 'H%d' % cl
                      vop('dve', v4(tab1), creb, phr, ALU.mult, ['Cre', 'PHr'], ['tab1'])
                      vop('pool', v4(tab2), cimb, phi, ALU.mult, ['Cim', 'PHi'], ['tab2'])
                      vop('dve', HR[:], tab1[:], tab2[:], ALU.subtract, ['tab1', 'tab2'], [hk])
                      vop('dve', v4(tab1), creb, phi, ALU.mult, ['Cre', 'PHi'], ['tab1'])
                      vop('pool', v4(tab2), ncimb, phr, ALU.mult, ['nCim', 'PHr'], ['tab2'])
                      vop('dve', HI[:], tab2[:], tab1[:], ALU.subtract, ['tab1', 'tab2'], [hk])
                      sub_stop(5)
                      KB = KBs[cl]
                      kk = 'KB%d' % cl
                      for ci_, (csrc, ck) in enumerate(((Cre, 'Cre'), (nCim, 'nCim'))):
                          for di_ in range(2):
                              s.op('dve' if di_ else 'pool',
                                   lambda e: e.tensor_scalar(out=Cmk[ci_ * 2 + di_][:], in0=csrc[:], scalar1=mkf[:, di_:di_ + 1],
                                                             scalar2=None, op0=ALU.mult), r=[ck, 'mkf'], w=['Cmk'])
                      for dl in range(-7, 8):
                          terms = []
                          if dl >= 0:
                              terms.append((0, 7 - dl))
                          if dl <= 0:
                              terms.append((1, -dl))
                          n = 0
                          for (di_, jj) in terms:
                              for (ab, ci_, ka) in ((ABr, 0, 'ABr'), (ABi, 1, 'ABi')):
                                  last = (n == 2 * len(terms) - 1)
                                  s.op('pe', lambda e: e.matmul(pkb[:], lhsT=ab[:, jj, :], rhs=Cmk[ci_ * 2 + di_][:],
                                                                start=(n == 0), stop=last),
                                       r=[ka, 'Cmk'], w=['pkb'], sig=last)
                                  n += 1
                          s.op('dve', lambda e: e.tensor_tensor(out=KB[:, dl + 7, :], in0=pkb[:], in1=blockmask[:], op=ALU.mult),
                               r=['pkb', 'blockmask'], w=[kk])
                      s.op('dve', lambda e: e.scalar_tensor_tensor(out=KB[:, 7, :], in0=ident[:], scalar=dT[:, ct:ct + 1],
                                                                   in1=KB[:, 7, :], op0=ALU.mult, op1=ALU.add),
                           r=[kk, 'ident', 'dT'], w=[kk])
                      sub_stop(6)
                      for j in range(8):
                          for ri, (ab, ka) in enumerate(((ABr, 'ABr'), (ABi, 'ABi'))):
                              s.op('pe', lambda e: e.transpose(pT3[:], ab[:, j, :], ident[:]), r=[ka, 'ident'], w=['pT3'])
                              copy_on(evac_eng(), ABT[:, j, ri, :], pT3[:], ['pT3'], ['ABT'])
                      sub_stop(7)
                      for g in range(8):
                          gl = g
                          if g % 2 == 0:
                              s.op('act', lambda e: e.activation(out=GTg[:].rearrange("p j r m -> p (j r m)"),
                                                                 in_=ABT[:].rearrange("p j r m -> p (j r m)"),
                                                                 func=AF.Identity, scale=rowmask[:, g:g + 1]),
                                   r=['ABT', 'rowmask'], w=['GTg'])
                          else:
                              s.op('dve', lambda e: e.tensor_scalar(out=GTg[:].rearrange("p j r m -> p (j r m)"),
                                                                    in0=ABT[:].rearrange("p j r m -> p (j r m)"),
                                                                    scalar1=rowmask[:, g:g + 1], scalar2=None, op0=ALU.mult),
                                   r=['ABT', 'rowmask'], w=['GTg'])
                          for ri, (Z, zk) in enumerate(((Zr, 'Zr'), (Zi, 'Zi'))):
                              pp = pst[ri]
                              pk = 'pst%d' % ri
                              for j in range(8):
                                  s.op('pe', lambda e: e.matmul(pp[:], lhsT=GTg[:, j, ri, :],
                                                                rhs=uT[cl][:].rearrange("p (c j) -> p j c", j=8)[:, j, :],
                                                                start=(j == 0), stop=(j == 7)),
                                       r=['GTg', uk], w=[pk], sig=(j == 7))
                              copy_on('act', Z[0:64, gl, 1:256], pp[0:64, 0:255], [pk], [zk + 'f'])
                              copy_on('dve', Z[64:128, gl, 0:255], pp[64:128, 1:256], [pk], [zk + 'b'])
                  sub_stop(8)
                  a8r = Akr[:, 8, G0:G0 + 8]
                  a8i = Aki[:, 8, G0:G0 + 8]
                  a16r = c8r[:, G0:G0 + 8]
                  a16i = c8i[:, G0:G0 + 8]
                  Zr4 = Zr[:].rearrange("p g (b i) -> p g b i", i=16)
                  Zi4 = Zi[:].rearrange("p g (b i) -> p g b i", i=16)

                  def cmuladd(eng, o, dr, di, sr, si, ar, ai, tms, zkeys, akeys):
                      m1, m2, m3, m4 = tms
                      kr, ki_ = zkeys
                      vop(eng, m1, sr, ar, ALU.mult, [kr, akeys[0]], ['m1' + o])
                      vop(eng, m2, si, ai, ALU.mult, [ki_, akeys[1]], ['m2' + o])
                      vop(eng, m3, sr, ai, ALU.mult, [kr, akeys[1]], ['m3' + o])
                      vop(eng, m4, si, ar, ALU.mult, [ki_, akeys[0]], ['m4' + o])
                      vop(eng, m1, m1, m2, ALU.subtract, ['m1' + o, 'm2' + o], ['m1' + o])
                      vop(eng, m3, m3, m4, ALU.add, ['m3' + o, 'm4' + o], ['m3' + o])
                      vop(eng, dr, dr, m1, ALU.add, [kr, 'm1' + o], [kr])
                      vop(eng, di, di, m3, ALU.add, [ki_, 'm3' + o], [ki_])

                  for st_ in range(15):
                      for (eng, psl, o) in (('dve', slice(0, 64), 'f'), ('pool', slice(64, 128), 'b')):
                          if o == 'f':
                              src, dst = st_, st_ + 1
                          else:
                              src, dst = 15 - st_, 14 - st_
                          ab_r = a8r[psl, :].unsqueeze(2).to_broadcast([64, 8, 16])
                          ab_i = a8i[psl, :].unsqueeze(2).to_broadcast([64, 8, 16])
                          cmuladd(eng, o, Zr4[psl, :, :, dst], Zi4[psl, :, :, dst], Zr4[psl, :, :, src], Zi4[psl, :, :, src],
                                  ab_r, ab_i, [t_[psl, :, :] for t_ in tm], ('Zr' + o, 'Zi' + o), ('Akr', 'Aki'))
                  for (eng, psl, o) in (('dve', slice(0, 64), 'f'), ('pool', slice(64, 128), 'b')):
                      for (Cn, Z4, kz, kc) in ((Cnr, Zr4, 'Zr' + o, 'Cnr' + o), (Cni, Zi4, 'Zi' + o, 'Cni' + o)):
                          if o == 'f':
                              s.op(eng, lambda e: e.memset(Cn[psl, :, 0:1], 0.0), w=[kc])
                              s.op(eng, lambda e: e.tensor_copy(out=Cn[psl, :, 1:16], in_=Z4[psl, :, 0:15, 15]), r=[kz], w=[kc])
                          else:
                              s.op(eng, lambda e: e.memset(Cn[psl, :, 15:16], 0.0), w=[kc])
                              s.op(eng, lambda e: e.tensor_copy(out=Cn[psl, :, 0:15], in_=Z4[psl, :, 1:16, 0]), r=[kz], w=[kc])
                  for st_ in range(14):
                      for (eng, psl, o) in (('dve', slice(0, 64), 'f'), ('pool', slice(64, 128), 'b')):
                          if o == 'f':
                              src, dst = st_ + 1, st_ + 2
                          else:
                              src, dst = 14 - st_, 13 - st_
                          cmuladd(eng, o, Cnr[psl, :, dst], Cni[psl, :, dst], Cnr[psl, :, src], Cni[psl, :, src],
                                  a16r[psl, :], a16i[psl, :], [t_[psl, :, 0] for t_ in tm], ('Cnr' + o, 'Cni' + o), ('c8r', 'c8i'))
                  T1 = ZRr[:].rearrange("p g (b i) -> p g b i", i=16)
                  T2 = ZRi[:].rearrange("p g (b i) -> p g b i", i=16)
                  ZRr4 = ZRr[:].rearrange("p g (b i) -> p g b i", i=16)
                  ZRi4 = ZRi[:].rearrange("p g (b i) -> p g b i", i=16)
                  prb = Ppr[:, :, G0:G0 + 8].rearrange("p i g -> p g i").unsqueeze(2).to_broadcast([128, 8, 16, 16])
                  pib = Ppi[:, :, G0:G0 + 8].rearrange("p i g -> p g i").unsqueeze(2).to_broadcast([128, 8, 16, 16])
                  crb_ = Cnr[:].unsqueeze(3).to_broadcast([128, 8, 16, 16])
                  cib_ = Cni[:].unsqueeze(3).to_broadcast([128, 8, 16, 16])
                  kC = ['Cnrf', 'Cnrb', 'Cnif', 'Cnib']
                  vop('dve', T1, prb, crb_, ALU.mult, ['Ppr'] + kC, ['ZRr'])
                  vop('pool', T2, pib, cib_, ALU.mult, ['Ppi'] + kC, ['ZRi'])
                  vop('dve', T1, T1, T2, ALU.subtract, ['ZRr', 'ZRi'], ['ZRr'])
                  vop('dve', ZRr4, Zr4, T1, ALU.add, ['Zrf', 'Zrb', 'ZRr'], ['ZRr'])
                  vop('pool', T2, prb, cib_, ALU.mult, ['Ppr'] + kC, ['ZRi'])
                  vop('dve', Zr4, pib, crb_, ALU.mult, ['Ppi'] + kC, ['Zrf', 'Zrb'])
                  vop('pool', T2, T2, Zr4, ALU.add, ['ZRi', 'Zrf', 'Zrb'], ['ZRi'])
                  vop('pool', ZRi4, Zi4, T2, ALU.add, ['Zif', 'Zib', 'ZRi'], ['ZRi'])
                  ZrR = ZRr[:]
                  ZiR = ZRi[:]
                  for cl in (gb % 2,):
                      ct = gb
                      HR, HI = Hs[cl]
                      hk = 'H%d' % cl
                      KB = KBs[cl]
                      kk = 'KB%d' % cl
                      uk = 'uT%d' % cl
                      for i in range(8):
                          for xi, Hx in enumerate((HR, HI)):
                              s.op('pool' if xi else 'dve',
                                   lambda e: e.tensor_tensor(out=HM[:, :, xi, :], in0=colmask[:],
                                                             in1=Hx[:, i, :].unsqueeze(1).to_broadcast([128, 8, 128]), op=ALU.mult),
                                   r=[hk, 'colmask'], w=['GTg'])
                          pp = py3[i % 2]
                          pk = 'py%d' % (i % 2)
                          nmm = 8 + 16
                          n = 0
                          for j in range(8):
                              s.op('pe', lambda e: e.matmul(pp[:], lhsT=KB[:, (i - j) + 7, :],
                                                            rhs=uT[cl][:].rearrange("p (c j) -> p j c", j=8)[:, j, :],
                                                            start=(n == 0), stop=False),
                                   r=[kk, uk], w=[pk], sig=False)
                              n += 1
                          for g in range(8):
                              gl = g
                              for xi, ZR in enumerate((ZrR, ZiR)):
                                  last = (n == nmm - 1)
                                  s.op('pe', lambda e: e.matmul(pp[:], lhsT=HM[:, g, xi, :], rhs=ZR[:, gl, 0:256],
                                                                start=False, stop=last),
                                       r=['GTg', 'ZRr', 'ZRi'], w=[pk], sig=last)
                                  n += 1
                          copy_on(evac_eng(), yT[:].rearrange("p (c j) -> p j c", j=8)[:, i, :], pp[:], [pk], ['yT'])
                      s.dma('sp', ymixT[ct * 128:(ct + 1) * 128, :], yT[:], r=['yT'], w=['ymixT'])
              s.barrier()
              stop_after(3)

          for st in phase(4):
              def sb(name, shape, dt=F32):
                  return st.enter_context(nc.sbuf_tensor(name, shape, dt))

              def ps(name, shape, dt=F32):
                  return st.enter_context(nc.psum_tensor(name, shape, dt))
              bias = sb("p4_bias", [128, 3, 16, 128])
              dq = sb("p4_dq", [128, 128])
              es = sb("p4_es", [128, 16])
              kT = sb("p4_kT", [128, T], F32R)
              vTt = sb("p4_vT", [128, T])
              V = sb("p4_V", [128, 16, 128], F32R)
              qT = sb("p4_qT", [128, 4, T], F32R)
              tmp = [sb("p4_tmp%d" % i, [128, 512]) for i in range(2)]
              pTt = [sb("p4_pT%d" % i, [128, 512], F32R) for i in range(2)]
              den = sb("p4_den", [128, 512])
              yo = [sb("p4_yo%d" % i, [128, 512]) for i in range(2)]
              ptv = ps("p4_ptv", [128, 128])
              psT = [ps("p4_psT%d" % i, [128, 512]) for i in range(3)]
              po = ps("p4_po", [128, 512])
              prs = ps("p4_prs", [128, 512])
              s.op('pool', lambda e: e.iota(dq[:], pattern=[[1, 128]], base=0, channel_multiplier=-1,
                                            allow_small_or_imprecise_dtypes=True), w=['dq'])
              adq = sb("p4_adq", [128, 128])
              s.op('dve', lambda e: e.tensor_scalar(out=adq[:], in0=dq[:], scalar1=-1.0, scalar2=None, op0=ALU.mult), r=['dq'], w=['adq'])
              s.op('dve', lambda e: e.tensor_tensor(out=adq[:], in0=adq[:], in1=dq[:], op=ALU.max), r=['dq', 'adq'], w=['adq'])
              for h in range(16):
                  slope = 2.0 ** (-8.0 * (h + 1) / 16.0)
                  s.op('dve', lambda e: e.tensor_scalar(out=bias[:, 0, h, :], in0=dq[:], scalar1=128.0, scalar2=-slope,
                                                        op0=ALU.add, op1=ALU.mult), r=['dq'], w=['bias'])
                  s.op('pool', lambda e: e.affine_select(out=bias[:, 0, h, :], in_=bias[:, 0, h, :], pattern=[[-1, 128]],
                                                         compare_op=ALU.is_ge, fill=-1e30, base=0, channel_multiplier=1),
                       r=['bias'], w=['bias'])
                  s.op('dve', lambda e: e.tensor_scalar(out=bias[:, 1, h, :], in0=adq[:], scalar1=-slope, scalar2=None,
                                                        op0=ALU.mult), r=['adq'], w=['bias'])
                  s.op('dve', lambda e: e.tensor_scalar(out=bias[:, 2, h, :], in0=dq[:], scalar1=-128.0, scalar2=slope,
                                                        op0=ALU.add, op1=ALU.mult), r=['dq'], w=['bias'])
                  s.op('pool', lambda e: e.affine_select(out=bias[:, 2, h, :], in_=bias[:, 2, h, :], pattern=[[1, 128]],
                                                         compare_op=ALU.is_ge, fill=-1e30, base=0, channel_multiplier=-1),
                       r=['bias'], w=['bias'])
              s.dma('sp', es[:], sink[0, :].partition_broadcast(128), w=['es'])
              s.op('act', lambda e: e.activation(out=es[:], in_=es[:], func=AF.Exp), r=['es'], w=['es'])
              scale = 128.0 ** -0.5
              it = 0
              for kh in range(4):
                  s.dma('sp', kT[:], projT[(32 + kh) * 128:(33 + kh) * 128, :], r=['projT'], w=['kT'])
                  s.dma('act', vTt[:], projT[(36 + kh) * 128:(37 + kh) * 128, :].bitcast(F32), r=['projT'], w=['vTt'])
                  for hh in range(4):
                      m = 16 + kh * 4 + hh
                      s.dma('sp' if hh % 2 else 'act', qT[:, hh, :], projT[m * 128:(m + 1) * 128, :], r=['projT'], w=['qT'])
                  for kb in range(16):
                      s.op('pe', lambda e: e.transpose(ptv[:], vTt[:, kb * 128:(kb + 1) * 128], ident[:]),
                           r=['vTt', 'ident'], w=['ptv'])
                      copy_on(evac_eng(), V[:, kb, :], ptv[:], ['ptv'], ['V'])
                  for n in range(16):
                      rels = [r_ for r_ in (-1, 0, 1) if 0 <= n + r_ < 16]
                      for ri_, rel in enumerate(rels):
                          kb = n + rel
                          pS = psT[it % 3]
                          pk = 'psT%d' % (it % 3)
                          tm = tmp[it % 2]
                          tk = 'tmp%d' % (it % 2)
                          pt_ = pTt[it % 2]
                          ptk = 'pTt%d' % (it % 2)
                          it += 1
                          s.op('pe', lambda e: e.matmul(pS[:], lhsT=kT[:, kb * 128:(kb + 1) * 128],
                                                        rhs=qT[:, :, n * 128:(n + 1) * 128], start=True, stop=True),
                               r=['kT', 'qT'], w=[pk])
                          s.op('dve', lambda e: e.scalar_tensor_tensor(
                              out=tm[:].rearrange("p (h q) -> p h q", h=4), in0=pS[:].rearrange("p (h q) -> p h q", h=4),
                              scalar=scale, in1=bias[:, rel + 1, kh * 4:(kh + 1) * 4, :], op0=ALU.mult, op1=ALU.add),
                              r=[pk, 'bias'], w=[tk])
                          s.op('act', lambda e: e.activation(out=pt_[:], in_=tm[:], func=AF.Exp), r=[tk], w=[ptk])
                          first = (ri_ == 0)
                          last = (ri_ == len(rels) - 1)
                          s.op('pe', lambda e: e.matmul(po[:], lhsT=V[:, kb, :], rhs=pt_[:], start=first, stop=last),
                               r=['V', ptk], w=['po'])
                          s.op('pe', lambda e: e.matmul(prs[:], lhsT=onesR[:], rhs=pt_[:], start=first, stop=last),
                               r=['onesR', ptk], w=['prs'])
                      yb = yo[n % 2]
                      yk = 'yo%d' % (n % 2)
                      s.op('dve', lambda e: e.tensor_tensor(out=den[:].rearrange("p (h q) -> p h q", h=4),
                                                            in0=prs[:].rearrange("p (h q) -> p h q", h=4),
                                                            in1=es[:, kh * 4:(kh + 1) * 4].unsqueeze(2).to_broadcast([128, 4, 128]),
                                                            op=ALU.add), r=['prs', 'es'], w=['den'])
                      s.op('dve', lambda e: e.reciprocal(out=den[:], in_=den[:]), r=['den'], w=['den'])
                      s.op('dve', lambda e: e.tensor_tensor(out=yb[:], in0=po[:], in1=den[:], op=ALU.mult),
                           r=['po', 'den'], w=[yk])
                      s.dma('pool', ymixT[2048 + kh * 512:2048 + (kh + 1) * 512, n * 128:(n + 1) * 128].rearrange("(h p) q -> p h q", p=128),
                            yb[:].rearrange("p (h q) -> p h q", h=4), r=[yk], w=['ymixT'])
              s.barrier()
              stop_after(4)

          def layer_norm(st_sb, r_t, sq_t, rkey, sqkey, pstat, gT, bT, gk, bk):
              stat = st_sb
              s.op('pe', lambda e: None, r=[], w=[]) if False else None
              for j in range(32):
                  s.op('pe', lambda e: e.matmul(pstat[:], lhsT=onesM[:], rhs=r_t[:, j, :], start=(j == 0), stop=(j == 31)),
                       r=['onesM', rkey], w=['pstat'], sig=(j == 31))
              s.op('dve', lambda e: e.tensor_tensor(out=r_t[:], in0=r_t[:], in1=pstat[:].unsqueeze(1).to_broadcast([128, 32, 256]),
                                                    op=ALU.subtract), r=[rkey, 'pstat'], w=[rkey])
              s.op('pool', lambda e: e.tensor_tensor(out=sq_t[:], in0=r_t[:], in1=r_t[:], op=ALU.mult), r=[rkey], w=[sqkey])
              for j in range(32):
                  s.op('pe', lambda e: e.matmul(pstat[:], lhsT=onesM[:], rhs=sq_t[:, j, :], start=(j == 0), stop=(j == 31)),
                       r=['onesM', sqkey], w=['pstat'], sig=(j == 31))
              s.op('act', lambda e: e.activation(out=stat[:], in_=pstat[:], func=AF.Sqrt, bias=epsc[:, 0:1], scale=1.0),
                   r=['pstat', 'epsc'], w=['stat'])
              s.op('dve', lambda e: e.reciprocal(out=stat[:], in_=stat[:]), r=['stat'], w=['stat'])
              s.op('dve', lambda e: e.tensor_tensor(out=r_t[:], in0=r_t[:], in1=stat[:].unsqueeze(1).to_broadcast([128, 32, 256]),
                                                    op=ALU.mult), r=[rkey, 'stat'], w=[rkey])
              s.op('pool', lambda e: e.tensor_tensor(out=r_t[:], in0=r_t[:], in1=gT[:].unsqueeze(2).to_broadcast([128, 32, 256]),
                                                     op=ALU.mult), r=[rkey, gk], w=[rkey])
              s.op('dve', lambda e: e.tensor_tensor(out=r_t[:], in0=r_t[:], in1=bT[:].unsqueeze(2).to_broadcast([128, 32, 256]),
                                                    op=ALU.add), r=[rkey, bk], w=[rkey])

          epsc = gsb("epsc", [128, 1])
          s.op('pool', lambda e: e.memset(epsc[:], EPS), w=['epsc'])

          for st in phase(5):
              def sb(name, shape, dt=F32):
                  return st.enter_context(nc.sbuf_tensor(name, shape, dt))

              def ps(name, shape, dt=F32):
                  return st.enter_context(nc.psum_tensor(name, shape, dt))
              A = sb("p5_A", [128, 32, 256])
              B = sb("p5_B", [128, D])
              mixR = sb("p5_mixR", [128, 32, 256], F32R)
              R = sb("p5_R", [128, 32, 256])
              wo = [sb("p5_wo%d" % i, [128, 32, 128], F32R) for i in range(2)]
              wg = [sb("p5_wg%d" % i, [128, 16, 128], F32R) for i in range(2)]
              gt = sb("p5_gt", [128, 256])
              stat = sb("p5_stat", [128, 256])
              wr = sb("p5_wr", [128, 32, 72])
              brB = sb("p5_brB", [128, 72])
              lg = sb("p5_lg", [128, 72])
              sm = [sb("p5_sm%d" % i, [128, 8]) for i in range(6)]
              sc = [sb("p5_sc%d" % i, [128, 1]) for i in range(8)]
              e3 = sb("p5_e3", [128, 8, 8])
              pacc = [ps("p5_acc%d" % i, [128, 256]) for i in range(2)]
              pstat = ps("p5_pstat", [128, 256])
              ptr = [ps("p5_ptr%d" % i, [128, 128]) for i in range(2)]
              plg = ps("p5_plg", [128, 72])
              s.dma('sp', wr[:], w_r.rearrange("(j p) n -> p j n", p=128), w=['wr'])
              s.dma('sp', brB[:], b_r[0, :].partition_broadcast(128), w=['brB'])
              ymv = ymixT.rearrange("(c p) t -> p c t", p=128)
              wgv = w_glu.rearrange("(k p) m -> p k m", p=128)
              wov = w_out.rearrange("(k p) m -> p k m", p=128)
              x1v = x1T.rearrange("(j p) t -> p j t", p=128)
              gyT = sb("p5_gy", [128, 16, 256], F32R)
              gyR = gyT[:]
              gyF = B[:, 0:16 * 256].rearrange("p (k t) -> p k t", k=16)
              tcount = 0
              for tq in range(8):
                  tsl = slice(tq * 256, (tq + 1) * 256)
                  s.dma('sp', A[:], ymv[:, :, tsl], r=['ymixT'], w=['A'])
                  s.op('act', lambda e: e.activation(out=gyR, in_=A[:, 0:16, :], func=AF.Gelu), r=['A'], w=['gy'])
                  for m in range(16):
                      wt = wg[m % 2]
                      wk = 'wg%d' % (m % 2)
                      s.dma('act' if m % 2 else 'pool', wt[:], wgv[:, :, m * 128:(m + 1) * 128], w=[wk])
                      pa = pacc[m % 2]
                      pk = 'acc%d' % (m % 2)
                      for k in range(16):
                          s.op('pe', lambda e: e.matmul(pa[:], lhsT=wt[:, k, :], rhs=gyR[:, k, :], start=(k == 0), stop=(k == 15)),
                               r=[wk, 'gy'], w=[pk], sig=(k == 15))
                      s.op('act', lambda e: e.activation(out=gt[:], in_=pa[:], func=AF.Sigmoid, bias=bgluT[:, m:m + 1], scale=1.0),
                           r=[pk, 'bgluT'], w=['gt'])
                      s.op('dve', lambda e: e.tensor_tensor(out=mixR[:, m, :], in0=A[:, m, :], in1=gt[:], op=ALU.mult),
                           r=['A', 'gt'], w=['mixR'])
                  s.op('pool', lambda e: e.tensor_copy(out=mixR[:, 16:32, :], in_=A[:, 16:32, :]), r=['A'], w=['mixR'])
                  for tt in range(2):
                      t0 = tq * 256 + tt * 128
                      s.dma('sp', B[:], x[t0:t0 + 128, :], w=['B'])
                      for j in range(32):
                          pt = ptr[tcount % 2]
                          pk = 'ptr%d' % (tcount % 2)
                          tcount += 1
                          s.op('pe', lambda e: e.transpose(pt[:], B[:, j * 128:(j + 1) * 128], ident[:]), r=['B', 'ident'], w=[pk])
                          dst = R[:, j, tt * 128:(tt + 1) * 128]
                          if j % 2:
                              s.op('act', lambda e: e.mul(out=dst, in_=pt[:], mul=ALPHA) if False else
                                   e.activation(out=dst, in_=pt[:], func=AF.Identity, scale=ALPHA), r=[pk], w=['R'])
                          else:
                              s.op('dve', lambda e: e.tensor_scalar(out=dst, in0=pt[:], scalar1=ALPHA, scalar2=None, op0=ALU.mult),
                                   r=[pk], w=['R'])
                  for m in range(32):
                      wt = wo[m % 2]
                      wk = 'wo%d' % (m % 2)
                      s.dma('sp' if m % 2 else 'act', wt[:], wov[:, :, m * 128:(m + 1) * 128], w=[wk])
                      pa = pacc[m % 2]
                      pk = 'acc%d' % (m % 2)
                      for k in range(32):
                          s.op('pe', lambda e: e.matmul(pa[:], lhsT=wt[:, k, :], rhs=mixR[:, k, :], start=(k == 0), stop=(k == 31)),
                               r=[wk, 'mixR'], w=[pk], sig=(k == 31))
                      s.op('dve', lambda e: e.scalar_tensor_tensor(out=R[:, m, :], in0=pa[:], scalar=g1p[:, m:m + 1],
                                                                   in1=R[:, m, :], op0=ALU.mult, op1=ALU.add),
                           r=[pk, 'g1p', 'R'], w=['R'])
                  layer_norm(stat, R, A, 'R', 'A', pstat, ln1gT, ln1bT, 'ln1gT', 'ln1bT')
                  s.dma('sp', x1v[:, :, tsl], R[:], r=['R'], w=['x1T'])
                  s.op('dve', lambda e: e.tensor_tensor(out=A[:], in0=R[:], in1=sc2p[:].unsqueeze(2).to_broadcast([128, 32, 256]),
                                                        op=ALU.mult), r=['R', 'sc2p'], w=['A'])
                  s.op('pool', lambda e: e.tensor_tensor(out=A[:], in0=A[:], in1=sh2.unsqueeze(2).to_broadcast([128, 32, 256]),
                                                         op=ALU.add), r=['A', 'modT'], w=['A'])
                  for tt in range(2):
                      tile = tq * 2 + tt
                      t0 = tile * 128
                      for j in range(32):
                          pt = ptr[tcount % 2]
                          pk = 'ptr%d' % (tcount % 2)
                          tcount += 1
                          s.op('pe', lambda e: e.transpose(pt[:], A[:, j, tt * 128:(tt + 1) * 128], ident[:]), r=['A', 'ident'], w=[pk])
                          copy_on(evac_eng(), B[:].bitcast(F32R)[:, j * 128:(j + 1) * 128], pt[:], [pk], ['B'])
                      s.dma('sp', h2tm[t0:t0 + 128, :], B[:].bitcast(F32R), r=['B'], w=['h2tm'])
                      for j in range(32):
                          s.op('pe', lambda e: e.matmul(plg[:], lhsT=A[:, j, tt * 128:(tt + 1) * 128], rhs=wr[:, j, :],
                                                        start=(j == 0), stop=(j == 31)), r=['A', 'wr'], w=['plg'], sig=(j == 31))
                      s.op('dve', lambda e: e.tensor_tensor(out=lg[:], in0=plg[:], in1=brB[:], op=ALU.add), r=['plg', 'brB'], w=['lg'])
                      gmax, ngmax, gsum, m1, m2, dm, w1, w2 = sc
                      ge, sel, oh1, sel2, oh2, wtmp = sm
                      Gt = Gall[:, tile, :]
                      s.op('dve', lambda e: e.tensor_reduce(out=gmax[:], in_=lg[:, 0:8], axis=AX.X, op=ALU.max), r=['lg'], w=['rt'])
                      s.op('dve', lambda e: e.tensor_scalar(out=ngmax[:], in0=gmax[:], scalar1=-1.0, scalar2=None, op0=ALU.mult), r=['rt'], w=['rt'])
                      s.op('act', lambda e: e.activation(out=ge[:], in_=lg[:, 0:8], func=AF.Exp, bias=ngmax[:, 0:1], scale=1.0),
                           r=['lg', 'rt'], w=['rt2'])
                      s.op('dve', lambda e: e.tensor_reduce(out=gsum[:], in_=ge[:], axis=AX.X, op=ALU.add), r=['rt2'], w=['rt'])
                      s.op('dve', lambda e: e.reciprocal(out=gsum[:], in_=gsum[:]), r=['rt'], w=['rt'])
                      s.op('dve', lambda e: e.tensor_scalar(out=Gt, in0=lg[:, 0:8], scalar1=gmax[:, 0:1], scalar2=None, op0=ALU.is_equal),
                           r=['lg', 'rt'], w=['Gall'])
                      s.op('dve', lambda e: e.tensor_tensor(out=e3[:], in0=lg[:, 8:72].rearrange("p (g e) -> p g e", g=8),
                                                            in1=Gt.unsqueeze(2).to_broadcast([128, 8, 8]), op=ALU.mult),
                           r=['lg', 'Gall'], w=['e3'])
                      s.op('dve', lambda e: e.tensor_reduce(out=sel[:], in_=e3[:].rearrange("p g e -> p e g"), axis=AX.X, op=ALU.add),
                           r=['e3'], w=['rt'])
                      s.op('dve', lambda e: e.tensor_reduce(out=m1[:], in_=sel[:], axis=AX.X, op=ALU.max), r=['rt'], w=['rt'])
                      s.op('dve', lambda e: e.tensor_scalar(out=oh1[:], in0=sel[:], scalar1=m1[:, 0:1], scalar2=None, op0=ALU.is_equal), r=['rt'], w=['rt'])
                      s.op('dve', lambda e: e.scalar_tensor_tensor(out=sel2[:], in0=oh1[:], scalar=-1e30, in1=sel[:], op0=ALU.mult, op1=ALU.add), r=['rt'], w=['rt'])
                      s.op('dve', lambda e: e.tensor_reduce(out=m2[:], in_=sel2[:], axis=AX.X, op=ALU.max), r=['rt'], w=['rt'])
                      s.op('dve', lambda e: e.tensor_scalar(out=oh2[:], in0=sel2[:], scalar1=m2[:, 0:1], scalar2=None, op0=ALU.is_equal), r=['rt'], w=['rt'])
                      s.op('dve', lambda e: e.tensor_tensor(out=dm[:], in0=m1[:], in1=m2[:], op=ALU.subtract), r=['rt'], w=['rt'])
                      s.op('act', lambda e: e.activation(out=w1[:], in_=dm[:], func=AF.Sigmoid), r=['rt'], w=['rt3'])
                      s.op('dve', lambda e: e.tensor_scalar(out=w2[:], in0=w1[:], scalar1=-1.0, scalar2=1.0, op0=ALU.mult, op1=ALU.add), r=['rt3'], w=['rt'])
                      s.op('dve', lambda e: e.tensor_scalar(out=wtmp[:], in0=oh1[:], scalar1=w1[:, 0:1], scalar2=None, op0=ALU.mult), r=['rt', 'rt3'], w=['rt'])
                      s.op('dve', lambda e: e.scalar_tensor_tensor(out=wtmp[:], in0=oh2[:], scalar=w2[:, 0:1], in1=wtmp[:], op0=ALU.mult, op1=ALU.add), r=['rt'], w=['rt'])
                      s.op('dve', lambda e: e.tensor_scalar(out=WTall[:, tile, :], in0=wtmp[:], scalar1=gsum[:, 0:1], scalar2=None, op0=ALU.mult),
                           r=['rt'], w=['WTall'])
              if debug:
                  s.dma('sp', dbg_G, Gall[:].rearrange('p a b -> p (a b)'), r=['Gall'], w=['dbg_G'])
                  s.dma('sp', dbg_W, WTall[:].rearrange('p a b -> p (a b)'), r=['WTall'], w=['dbg_W'])
              s.barrier()
              stop_after(5)

          for st in phase(6):
              def sb(name, shape, dt=F32):
                  return st.enter_context(nc.sbuf_tensor(name, shape, dt))

              def ps(name, shape, dt=F32):
                  return st.enter_context(nc.psum_tensor(name, shape, dt))
              pb = [ps("p6_b%d" % i, [128, 512]) for i in range(8)]
              Pg = sb("p6_Pg", [128, 16, CAP], F32R)
              big = sb("p6_big", [128, 32 * CAP], F32R)
              hbT = sb("p6_hbT", [128, 32, CAP], F32R)
              WTB = sb("p6_WTB", [128, 8, CAP])
              wtT = sb("p6_wtT", [8, CAP])
              h2s = [sb("p6_h2s%d" % i, [128, 1024], F32R) for i in range(2)]
              wtl = [sb("p6_wt%d" % i, [128, 32, 128], F32R) for i in range(2)]
              sg = sb("p6_sg", [128, CAP])
              yst = [sb("p6_yst%d" % i, [128, 256], F32R) for i in range(2)]
              pp8 = sb("p6_pp8", [128, 8])
              for tt in range(16):
                  n = 0
                  for t2_ in range(tt + 1):
                      lh = onesF if t2_ < tt else ustrict
                      s.op('pe', lambda e: e.matmul(pb[0][:, 0:8], lhsT=lh[:], rhs=Gall[:, t2_, :], start=(n == 0), stop=(t2_ == tt)),
                           r=['Gall', 'onesF', 'ustrict'], w=['pb0'], sig=(t2_ == tt))
                      n += 1
                  s.op('dve', lambda e: e.tensor_tensor(out=pp8[:], in0=pb[0][:, 0:8], in1=Gall[:, tt, :], op=ALU.mult),
                       r=['pb0', 'Gall'], w=['pp8'])
                  s.op('dve', lambda e: e.tensor_reduce(out=posall[:, tt:tt + 1], in_=pp8[:], axis=AX.X, op=ALU.add),
                       r=['pp8'], w=['posall'])
              hTg = big[:, 0:32 * CAP].rearrange("p (j c) -> p j c", j=32)
              wd = big[:, 0:32 * 256].rearrange("p (k c) -> p k c", k=32)
              PgF = Pg[:].bitcast(F32)
              yav = yall
              wcount = 0
              ycount = 0
              for g in range(8):
                  for tt in range(16):
                      s.op('dve',
                           lambda e: e.tensor_scalar(out=Pg[:, tt, :], in0=iotaS[:], scalar1=posall[:, tt:tt + 1],
                                                     scalar2=Gall[:, tt, g:g + 1], op0=ALU.is_equal, op1=ALU.mult),
                           r=['iotaS', 'posall', 'Gall'], w=['Pg'])
                  for jb in range(4):
                      for tt in range(16):
                          hb_ = h2s[tt % 2]
                          hk = 'h2s%d' % (tt % 2)
                          s.dma('sp' if tt % 2 else 'act', hb_[:], h2tm[tt * 128:(tt + 1) * 128, jb * 1024:(jb + 1) * 1024],
                                r=['h2tm'], w=[hk])
                          for jj in range(8):
                              s.op('pe', lambda e: e.matmul(pb[jj][:, 0:CAP], lhsT=hb_[:, jj * 128:(jj + 1) * 128], rhs=Pg[:, tt, :],
                                                            start=(tt == 0), stop=(tt == 15)),
                                   r=[hk, 'Pg'], w=['pb%d' % jj], sig=(jj == 7))
                      for jj in range(8):
                          copy_on(evac_eng(), hTg[:, jb * 8 + jj, :], pb[jj][:, 0:CAP], ['pb%d' % jj], ['big'])
                  for tt in range(16):
                      s.op('pe', lambda e: e.matmul(pb[0][0:8, 0:CAP], lhsT=WTall[:, tt, :], rhs=PgF[:, tt, :], start=(tt == 0), stop=(tt == 15)),
                           r=['WTall', 'Pg'], w=['pb0'], sig=(tt == 15))
                  s.op('dve', lambda e: e.tensor_copy(out=wtT[:], in_=pb[0][0:8, 0:CAP]), r=['pb0'], w=['wtT'])
                  for e_ in range(8):
                      s.op('pe', lambda e: e.matmul(pb[1][:, 0:CAP], lhsT=sel8[:, e_, :], rhs=wtT[:], start=True, stop=True),
                           r=['sel8', 'wtT'], w=['pb1'])
                      copy_on(evac_eng(), WTB[:, e_, :], pb[1][:, 0:CAP], ['pb1'], ['WTB'])
                  for e_ in range(8):
                      ex = g * 8 + e_
                      for fc in range(4):
                          for gu, wsrc in enumerate((w_gate, w_up)):
                              wt = wtl[wcount % 2]
                              wk = 'wtl%d' % (wcount % 2)
                              s.dma(('sp', 'act', 'pool')[wcount % 3], wt[:],
                                    wsrc[ex].rearrange("(j p) f -> p j f", p=128)[:, :, fc * 128:(fc + 1) * 128], w=[wk])
                              wcount += 1
                              pa = pb[2 + gu]
                              pk = 'pb%d' % (2 + gu)
                              for j in range(32):
                                  s.op('pe', lambda e: e.matmul(pa[:, 0:CAP], lhsT=wt[:, j, :], rhs=hTg[:, j, :], start=(j == 0), stop=(j == 31)),
                                       r=[wk, 'big'], w=[pk], sig=(j == 31))
                          s.op('act', lambda e: e.activation(out=sg[:], in_=pb[2][:, 0:CAP], func=AF.Silu), r=['pb2'], w=['sg'])
                          s.op('dve', lambda e: e.tensor_tensor(out=sg[:], in0=sg[:], in1=pb[3][:, 0:CAP], op=ALU.mult), r=['sg', 'pb3'], w=['sg'])
                          s.op('pool', lambda e: e.tensor_tensor(out=hbT[:, e_ * 4 + fc, :], in0=sg[:], in1=WTB[:, e_, :], op=ALU.mult),
                               r=['sg', 'WTB'], w=['hbT'])
                  wdv = w_down[g * 8:(g + 1) * 8].rearrange("e (fc p) d -> p (e fc) d", p=128)
                  for cc in range(16):
                      s.dma('sp' if cc % 2 else 'act', wd, wdv[:, :, cc * 256:(cc + 1) * 256], w=['big'])
                      for st_ in range(3):
                          pa = pb[4 + (ycount % 2)]
                          pk = 'pb%d' % (4 + (ycount % 2))
                          for k in range(32):
                              s.op('pe', lambda e: e.matmul(pa[:, 0:256], lhsT=hbT[:, k, st_ * 128:(st_ + 1) * 128], rhs=wd[:, k, :],
                                                            start=(k == 0), stop=(k == 31)), r=['hbT', 'big'], w=[pk], sig=(k == 31))
                          yb = yst[ycount % 2]
                          yk = 'yst%d' % (ycount % 2)
                          ycount += 1
                          copy_on(evac_eng(), yb[:], pa[:, 0:256], [pk], [yk])
                          row0 = g * CAP + st_ * 128
                          s.dma('pool', yav[row0:row0 + 128, cc * 256:(cc + 1) * 256], yb[:], r=[yk], w=['yall'])
              s.barrier()
              stop_after(6)

          for st in phase(7):
              def sb(name, shape, dt=F32):
                  return st.enter_context(nc.sbuf_tensor(name, shape, dt))

              def ps(name, shape, dt=F32):
                  return st.enter_context(nc.psum_tensor(name, shape, dt))
              R = sb("p7_R", [128, 32, 256])
              SQ = sb("p7_SQ", [128, 32, 256])
              PT = sb("p7_PT", [128, 24, 256], F32R)
              yl = [sb("p7_yl%d" % i, [128, 24, 128], F32R) for i in range(2)]
              otok = [sb("p7_otok%d" % i, [128, D]) for i in range(2)]
              posB = sb("p7_posB", [128, 256])
              GB = sb("p7_GB", [128, 8, 256])
              dg = sb("p7_dg", [128, 128])
              eqt = sb("p7_eq", [128, 256])
              stat = sb("p7_stat", [128, 256])
              pbc = ps("p7_pbc", [128, 128])
              pacc = [ps("p7_acc%d" % i, [128, 256]) for i in range(2)]
              pstat = ps("p7_pstat", [128, 256])
              ptr = [ps("p7_ptr%d" % i, [128, 128]) for i in range(2)]
              x1v = x1T.rearrange("(j p) t -> p j t", p=128)
              ylv = yall.rearrange("(s p) d -> p s d", p=128)
              tcount = 0
              for tq in range(8):
                  tsl = slice(tq * 256, (tq + 1) * 256)
                  s.dma('sp', R[:], x1v[:, :, tsl], r=['x1T'], w=['R'])
                  s.op('act', lambda e: e.activation(out=R[:], in_=R[:], func=AF.Identity, scale=ALPHA), r=['R'], w=['R'])
                  for tt in range(2):
                      tile = tq * 2 + tt
                      csl = slice(tt * 128, (tt + 1) * 128)
                      s.op('dve', lambda e: e.tensor_scalar(out=dg[:], in0=ident[:], scalar1=posall[:, tile:tile + 1], scalar2=None, op0=ALU.mult),
                           r=['ident', 'posall'], w=['dg'])
                      s.op('pe', lambda e: e.matmul(pbc[:], lhsT=onesF[:], rhs=dg[:], start=True, stop=True), r=['onesF', 'dg'], w=['pbc'])
                      s.op('act', lambda e: e.copy(out=posB[:, csl], in_=pbc[:]), r=['pbc'], w=['posB'])
                      for g in range(8):
                          s.op('dve', lambda e: e.tensor_scalar(out=dg[:], in0=ident[:], scalar1=Gall[:, tile, g:g + 1], scalar2=None, op0=ALU.mult),
                               r=['ident', 'Gall'], w=['dg'])
                          s.op('pe', lambda e: e.matmul(pbc[:], lhsT=onesF[:], rhs=dg[:], start=True, stop=True), r=['onesF', 'dg'], w=['pbc'])
                          s.op('act', lambda e: e.copy(out=GB[:, g, csl], in_=pbc[:]), r=['pbc'], w=['GB'])
                  for g in range(8):
                      for s3 in range(3):
                          s.op('dve', lambda e: e.tensor_scalar(out=eqt[:], in0=posB[:], scalar1=iotaP[:, s3:s3 + 1], scalar2=None, op0=ALU.is_equal),
                               r=['posB', 'iotaP'], w=['eqt'])
                          s.op('pool', lambda e: e.tensor_tensor(out=PT[:, g * 3 + s3, :], in0=eqt[:], in1=GB[:, g, :], op=ALU.mult),
                               r=['eqt', 'GB'], w=['PT'])
                  for j in range(32):
                      yb = yl[j % 2]
                      yk = 'yl%d' % (j % 2)
                      s.dma('sp' if j % 2 else 'act', yb[:], ylv[:, :, j * 128:(j + 1) * 128], r=['yall'], w=[yk])
                      pa = pacc[j % 2]
                      pk = 'acc%d' % (j % 2)
                      for s3 in range(24):
                          s.op('pe', lambda e: e.matmul(pa[:], lhsT=yb[:, s3, :], rhs=PT[:, s3, :], start=(s3 == 0), stop=(s3 == 23)),
                               r=[yk, 'PT'], w=[pk], sig=(s3 == 23))
                      s.op('dve', lambda e: e.scalar_tensor_tensor(out=R[:, j, :], in0=pa[:], scalar=g2p[:, j:j + 1], in1=R[:, j, :],
                                                                   op0=ALU.mult, op1=ALU.add), r=[pk, 'g2p', 'R'], w=['R'])
                  layer_norm(stat, R, SQ, 'R', 'SQ', pstat, ln2gT, ln2bT, 'ln2gT', 'ln2bT')
                  for tt in range(2):
                      ob = otok[tt]
                      ok = 'otok%d' % tt
                      t0 = tq * 256 + tt * 128
                      for j in range(32):
                          pt = ptr[tcount % 2]
                          pk = 'ptr%d' % (tcount % 2)
                          tcount += 1
                          s.op('pe', lambda e: e.transpose(pt[:], R[:, j, tt * 128:(tt + 1) * 128], ident[:]), r=['R', 'ident'], w=[pk])
                          copy_on(evac_eng(), ob[:, j * 128:(j + 1) * 128], pt[:], [pk], [ok])
                      s.dma('sp', out[t0:t0 + 128, :], ob[:], r=[ok], w=['out'])
              s.barrier()
    except _Stop:
        pass
    s.finish()
    nc._sch = s
    return nc


_NC = None


def make_shared(inputs):
    f = lambda a: np.ascontiguousarray(np.asarray(a, dtype=np.float32))
    return {
        'w_ada': f(inputs['w_ada'][0]),
        'b_ada': f(inputs['b_ada'][0]).reshape(192, 128),
        'w_in': f(inputs['w_in'][0]),
        'lam_re': f(inputs['ssm_lam_re'][0]), 'lam_im': f(inputs['ssm_lam_im'][0]),
        'log_dt': f(inputs['ssm_log_dt'][0]),
        'b_re': f(inputs['ssm_b_re'][0]), 'b_im': f(inputs['ssm_b_im'][0]),
        'c_re': f(inputs['ssm_c_re'][0]), 'c_im': f(inputs['ssm_c_im'][0]),
        'ssm_d': f(inputs['ssm_d'][0]).reshape(16, 128),
        'w_glu': f(inputs['w_glu'][0]), 'b_glu': f(inputs['b_glu'][0]).reshape(16, 128),
        'sink': f(inputs['attn_sink'][0]).reshape(1, 16),
        'w_out': f(inputs['w_out'][0]),
        'ln1_g': f(inputs['ln1_g'][0]).reshape(32, 128), 'ln1_b': f(inputs['ln1_b'][0]).reshape(32, 128),
        'w_r': f(np.concatenate([inputs['w_router_group'][0], inputs['w_router_expert'][0]], axis=1)),
        'b_r': f(np.concatenate([inputs['b_router_group'][0], inputs['b_router_expert'][0]], axis=0)).reshape(1, 72),
        'w_gate': f(inputs['w_gate_e'][0]), 'w_up': f(inputs['w_up_e'][0]), 'w_down': f(inputs['w_down_e'][0]),
        'ln2_g': f(inputs['ln2_g'][0]).reshape(32, 128), 'ln2_b': f(inputs['ln2_b'][0]).reshape(32, 128),
    }


def kernel(**inputs):
    global _NC
    f = lambda a: np.ascontiguousarray(np.asarray(a, dtype=np.float32))
    x = f(inputs['x']); c = f(inputs['c'])
    shared = make_shared(inputs)
    if _NC is None:
        _NC = build()
    in_maps = []
    for r in range(8):
        b = r % 4
        m = dict(shared)
        m['x'] = f(x[b])
        m['c'] = f(c[b]).reshape(32, 128)
        in_maps.append(m)
    res = run_bass_kernel_spmd(_NC, in_maps, core_ids=list(range(8)))
    outs = [np.asarray(res.results[b]['out'], dtype=np.float32).reshape(T, D) for b in range(4)]
    return np.stack(outs, axis=0)
```
